# Optimizing a Trainium2 kernel written in Bass

```python
import math
import jax, jax.numpy as jnp
from jax import lax
import numpy as np

D_MODEL = 1024
BATCH = 4
SEQ = 4096
DEPTH = 4

N_MIXERS = 2
N_RET_LAYERS = (DEPTH + N_MIXERS - 1) // N_MIXERS
N_SB_LAYERS = DEPTH // N_MIXERS
PLE_DIM = 256
D_FF = 2816
FFN_RES_WEIGHT = 0.5
RET_HEADS = 4
RET_DK = D_MODEL // RET_HEADS
RET_QK = RET_HEADS * RET_DK
RET_DV = 2 * RET_DK
RET_V = RET_HEADS * RET_DV
RET_IN = 2 * RET_QK + 2 * RET_V
RET_CHUNK = 128
ROPE_BASE = 10000.0
GN_EPS = 1e-5
SB_HEADS = 8
SB_DH = D_MODEL // SB_HEADS
SB_WIDTH = SB_HEADS * SB_DH
SB_IN = 3 * SB_WIDTH
SB_BLOCK = 128
N_NORMS = 8
RMS_EPS = 1e-6

kernel_name = "hybrid_retention_stickbreaking_macaron_trunk"


def rmsnorm(x, g):
    xf = x.astype(jnp.float32)
    y = xf * lax.rsqrt(jnp.mean(xf * xf, axis=-1, keepdims=True) + RMS_EPS)
    return (y * g.astype(jnp.float32)).astype(x.dtype)


def swiglu(x, w_gate, w_up, w_down):
    return (jax.nn.silu(x @ w_gate) * (x @ w_up)) @ w_down


def rope(t, positions):
    half = t.shape[-1] // 2
    inv = ROPE_BASE ** (-jnp.arange(half, dtype=jnp.float32) / half)
    ang = positions.astype(jnp.float32)[..., None] * inv
    cos = jnp.cos(ang)[:, :, None, :]
    sin = jnp.sin(ang)[:, :, None, :]
    t1, t2 = t[..., :half], t[..., half:]
    return jnp.concatenate([t1 * cos - t2 * sin, t1 * sin + t2 * cos], axis=-1)


def retention(h, positions, w_in, gn_gain, w_out):
    B, S, _ = h.shape
    proj = h @ w_in
    q, k, v, g = jnp.split(proj, [RET_QK, 2 * RET_QK, 2 * RET_QK + RET_V], axis=-1)
    q = rope(q.astype(jnp.float32).reshape(B, S, RET_HEADS, RET_DK), positions)
    k = rope(k.astype(jnp.float32).reshape(B, S, RET_HEADS, RET_DK), positions) * (RET_DK ** -0.5)
    v = v.astype(jnp.float32).reshape(B, S, RET_HEADS, RET_DV)

    nc = S // RET_CHUNK
    def to_chunks(t):
        return t.reshape(B, nc, RET_CHUNK, RET_HEADS, -1).transpose(1, 0, 3, 2, 4)
    qc, kc, vc = to_chunks(q), to_chunks(k), to_chunks(v)

    log_gamma = jnp.log1p(-jnp.exp2(-5.0 - jnp.arange(RET_HEADS, dtype=jnp.float32)))
    idx = jnp.arange(RET_CHUNK, dtype=jnp.float32)
    rel = idx[:, None] - idx[None, :]
    inner_decay = jnp.where(rel[None] >= 0,
                            jnp.exp(jnp.maximum(rel, 0.0)[None] * log_gamma[:, None, None]),
                            0.0)
    xi = jnp.exp((idx + 1.0)[None, :] * log_gamma[:, None])
    zeta = jnp.exp((RET_CHUNK - 1.0 - idx)[None, :] * log_gamma[:, None])
    chunk_decay = jnp.exp(RET_CHUNK * log_gamma)

    def step(state, inp):
        qi, ki, vi = inp
        scores = jnp.einsum('bhid,bhjd->bhij', qi, ki) * inner_decay[None]
        o_inner = jnp.einsum('bhij,bhjv->bhiv', scores, vi)
        o_cross = jnp.einsum('bhid,bhdv->bhiv', qi, state) * xi[None, :, :, None]
        new_state = state * chunk_decay[None, :, None, None] + jnp.einsum(
            'bhjd,bhjv->bhdv', ki * zeta[None, :, :, None], vi)
        return new_state, o_inner + o_cross

    state0 = jnp.zeros((B, RET_HEADS, RET_DK, RET_DV), jnp.float32)
    _, o = lax.scan(step, state0, (qc, kc, vc))
    o = o.transpose(1, 0, 3, 2, 4).reshape(B, S, RET_HEADS, RET_DV)
    mu = jnp.mean(o, axis=-1, keepdims=True)
    var = jnp.mean(jnp.square(o - mu), axis=-1, keepdims=True)
    o = ((o - mu) * lax.rsqrt(var + GN_EPS)).reshape(B, S, RET_V) * gn_gain.astype(jnp.float32)
    o = jax.nn.silu(g.astype(jnp.float32)) * o
    return o.astype(h.dtype) @ w_out


def stick_breaking(h, w_in, w_out):
    B, S, _ = h.shape
    proj = h @ w_in
    q, k, v = jnp.split(proj, [SB_WIDTH, 2 * SB_WIDTH], axis=-1)
    def heads(t):
        return t.astype(jnp.float32).reshape(B, S, SB_HEADS, SB_DH).transpose(0, 2, 1, 3)
    q, k, v = heads(q), heads(k), heads(v)
    scale = SB_DH ** -0.5
    nb = S // SB_BLOCK
    qb = q.reshape(B, SB_HEADS, nb, SB_BLOCK, SB_DH).transpose(2, 0, 1, 3, 4)
    kpos = jnp.arange(S)

    def block(args):
        q_blk, b = args
        qpos = b * SB_BLOCK + jnp.arange(SB_BLOCK)
        z = jnp.einsum('bhqd,bhkd->bhqk', q_blk, k) * scale
        mask = (kpos[None, :] < qpos[:, None])[None, None]
        log_one_minus = jnp.where(mask, jax.nn.log_sigmoid(-z), 0.0)
        rem = lax.cumsum(log_one_minus, axis=3, reverse=True) - log_one_minus
        a = jnp.where(mask, jnp.exp(jax.nn.log_sigmoid(z) + rem), 0.0)
        return jnp.einsum('bhqk,bhkd->bhqd', a, v)

    o = lax.map(block, (qb, jnp.arange(nb)))
    o = o.transpose(1, 0, 3, 2, 4).reshape(B, S, SB_WIDTH)
    return o.astype(h.dtype) @ w_out


def setup_inputs(seed: int = 0) -> dict:
    key = jax.random.key(seed)
    ks = jax.random.split(key, 16)
    f32 = jnp.float32
    def w(k, shape, fan_in):
        return jax.random.normal(k, shape, f32) * (fan_in ** -0.5)
    x = jax.random.normal(ks[0], (BATCH, SEQ, D_MODEL), f32)
    p = jax.random.normal(ks[1], (DEPTH, BATCH, SEQ, PLE_DIM), f32)
    positions = jnp.broadcast_to(jnp.arange(SEQ, dtype=jnp.int32), (BATCH, SEQ))
    norm_gains = 1.0 + 0.05 * jax.random.normal(ks[2], (DEPTH, N_NORMS, D_MODEL), f32)
    ffn_w_gate = w(ks[3], (DEPTH, 2, D_MODEL, D_FF), D_MODEL)
    ffn_w_up = w(ks[4], (DEPTH, 2, D_MODEL, D_FF), D_MODEL)
    ffn_w_down = w(ks[5], (DEPTH, 2, D_FF, D_MODEL), D_FF)
    ret_w_in = w(ks[6], (N_RET_LAYERS, D_MODEL, RET_IN), D_MODEL)
    ret_gn_gain = 1.0 + 0.05 * jax.random.normal(ks[7], (N_RET_LAYERS, RET_V), f32)
    ret_w_out = w(ks[8], (N_RET_LAYERS, RET_V, D_MODEL), RET_V)
    sb_w_in = w(ks[9], (N_SB_LAYERS, D_MODEL, SB_IN), D_MODEL)
    sb_w_out = w(ks[10], (N_SB_LAYERS, SB_WIDTH, D_MODEL), SB_WIDTH)
    ple_w_gate = w(ks[11], (DEPTH, D_MODEL, D_MODEL), D_MODEL)
    ple_w_proj = w(ks[12], (DEPTH, PLE_DIM, D_MODEL), PLE_DIM)
    return {"x": x, "p": p, "positions": positions, "norm_gains": norm_gains,
            "ffn_w_gate": ffn_w_gate, "ffn_w_up": ffn_w_up, "ffn_w_down": ffn_w_down,
            "ret_w_in": ret_w_in, "ret_gn_gain": ret_gn_gain, "ret_w_out": ret_w_out,
            "sb_w_in": sb_w_in, "sb_w_out": sb_w_out,
            "ple_w_gate": ple_w_gate, "ple_w_proj": ple_w_proj}


def reference(x, p, positions, norm_gains, ffn_w_gate, ffn_w_up, ffn_w_down,
              ret_w_in, ret_gn_gain, ret_w_out, sb_w_in, sb_w_out,
              ple_w_gate, ple_w_proj):
    h = x
    for i in range(DEPTH):
        g = norm_gains[i]
        f = swiglu(rmsnorm(h, g[0]), ffn_w_gate[i, 0], ffn_w_up[i, 0], ffn_w_down[i, 0])
        h = h + FFN_RES_WEIGHT * rmsnorm(f, g[1])
        m_in = rmsnorm(h, g[2])
        j = i // N_MIXERS
        if i % N_MIXERS == 0:
            m = retention(m_in, positions, ret_w_in[j], ret_gn_gain[j], ret_w_out[j])
        else:
            m = stick_breaking(m_in, sb_w_in[j], sb_w_out[j])
        h = h + rmsnorm(m, g[3])
        f = swiglu(rmsnorm(h, g[4]), ffn_w_gate[i, 1], ffn_w_up[i, 1], ffn_w_down[i, 1])
        h = h + FFN_RES_WEIGHT * rmsnorm(f, g[5])
        gate = jax.nn.sigmoid(rmsnorm(h, g[6]) @ ple_w_gate[i])
        e = p[i] @ ple_w_proj[i]
        h = h + rmsnorm(gate * e, g[7])
    return h
```

```python
import contextlib
import math
import numpy as np
import concourse.bass as bass
import concourse.mybir as mybir
from concourse.bass_utils import run_bass_kernel_spmd

F32, BF16, I32 = mybir.dt.float32, mybir.dt.bfloat16, mybir.dt.int32
AF = mybir.ActivationFunctionType
ALU = mybir.AluOpType

D = 1024
SEQ = 4096
BATCH = 4
DEPTH = 4
T = 2048
NG = 4
DFF = 2816
NFT = 22
PLE = 256
RMS_EPS = 1e-6
GN_EPS = 1e-5
RH, RDK, RDV = 4, 256, 512
SH, SDH = 8, 128
NEG = -30000.0
GAMMA = [1.0 - 2.0 ** (-5.0 - h) for h in range(RH)]


def layer_plan(kind):
    pl = []
    for f in range(2):
        pl += [(f"g{f}", 'A', 8, 256, 11), (f"u{f}", 'A', 8, 256, 11), (f"d{f}", 'B', 22, 128, 8)]
    if kind == 'ret':
        pl += [("min", 'A', 8, 256, 24), ("mout", 'A', 16, 128, 8)]
    else:
        pl += [("min", 'A', 8, 256, 12), ("mout", 'A', 8, 256, 4)]
    pl += [("pg", 'A', 8, 256, 4), ("pp", 'A', 2, 1024, 1)]
    return pl


def layer_offsets(kinds):
    na = nb = 0
    out = []
    for kind in kinds:
        d = {}
        for name, cls, kt, cw, n in layer_plan(kind):
            if cls == 'A':
                d[name] = (cls, na, kt, cw)
                na += n
            else:
                d[name] = (cls, nb, kt, cw)
                nb += n
        out.append(d)
    return out, na, nb


def pack_w(W, KT, CW):
    K, N = W.shape
    assert K == KT * 128 and N % CW == 0
    return np.ascontiguousarray(
        W.reshape(KT, 128, N // CW, CW).transpose(2, 1, 0, 3)).reshape(N // CW, 128, KT * CW)


class Buf:
    __slots__ = ("lw", "rd", "rdd", "ro")

    def __init__(self):
        self.lw = None
        self.rd = {}
        self.rdd = []
        self.ro = False


class TV:
    __slots__ = ("buf", "ap")

    def __init__(self, buf, ap):
        self.buf = buf
        self.ap = ap

    def __getitem__(self, idx):
        return TV(self.buf, self.ap[idx])


class Op:
    __slots__ = ("eng", "fn", "deps", "kind", "target", "val", "slot", "dval")

    def __init__(self, eng, fn, deps, kind):
        self.eng = eng
        self.fn = fn
        self.deps = deps
        self.kind = kind
        self.target = False
        self.val = 0
        self.slot = None
        self.dval = 0


ENGS = ("pe", "act", "dve", "pool", "sp")
NSLOT = {"sp": 24, "act": 8, "pool": 8}


class K:
    def __init__(self, nc):
        self.nc = nc
        self.streams = {e: [] for e in ENGS}
        self.fence = None
        self.dma_since_fence = []
        self.slot_ctr = {q: 0 for q in NSLOT}
        self.slot_last = {}
        self.slot_cnt = {}
        self.cc_cnt = 0
        self.out_dmas = []

    def op(self, eng, fn, reads=(), writes=(), kind="c", nofence=False):
        deps = set()
        for r in reads:
            if r.buf.lw is not None:
                deps.add(r.buf.lw)
        for w in writes:
            b = w.buf
            if b.lw is not None:
                deps.add(b.lw)
            deps.update(b.rd.values())
            deps.update(b.rdd)
        if self.fence is not None and not nofence:
            deps.add(self.fence)
        o = Op(eng, fn, deps, kind)
        if kind == "d":
            q = eng
            s = self.slot_ctr[q] % NSLOT[q]
            self.slot_ctr[q] += 1
            key = (q, s)
            if key in self.slot_last:
                deps.add(self.slot_last[key])
            self.slot_last[key] = o
            self.slot_cnt[key] = self.slot_cnt.get(key, 0) + 1
            o.slot = key
            o.dval = 16 * self.slot_cnt[key]
            self.dma_since_fence.append(o)
        elif kind == "cc":
            self.cc_cnt += 1
            o.dval = self.cc_cnt
            self.dma_since_fence.append(o)
        for r in reads:
            b = r.buf
            if b.ro:
                continue
            if kind == "c":
                b.rd[eng] = o
            else:
                b.rdd.append(o)
        for w in writes:
            b = w.buf
            b.lw = o
            b.rd = {}
            b.rdd = []
        deps.discard(o)
        self.streams[eng].append(o)
        return o

    def barrier(self, scratch):
        deps = set(self.dma_since_fence)
        for e in ENGS:
            for o in reversed(self.streams[e]):
                if o.kind == "c":
                    deps.add(o)
                    break
        if self.fence is not None:
            deps.add(self.fence)
        o = Op("dve", lambda e: e.memset(scratch.ap, 0.0), deps, "c")
        self.streams["dve"].append(o)
        self.fence = o
        self.dma_since_fence = []

    def emit(self, stack):
        nc = self.nc
        for e in ENGS:
            for o in self.streams[e]:
                for d in o.deps:
                    if d.kind == "c":
                        d.target = True
        sem = {}
        for e in ENGS:
            sem[e] = stack.enter_context(nc.semaphore("s_" + e))
            n = 0
            for o in self.streams[e]:
                if o.kind == "c" and o.target:
                    n += 1
                    o.val = n
        for q, ns in NSLOT.items():
            for s in range(ns):
                sem[(q, s)] = stack.enter_context(nc.semaphore(f"d_{q}{s}"))
        sem["cc"] = stack.enter_context(nc.semaphore("s_cc"))
        final = list(self.out_dmas)
        block = stack.enter_context(nc.Block())

        def run(ename, e):
            seen = {}
            for o in self.streams[ename]:
                for d in o.deps:
                    if d.kind == "c":
                        if d.eng == ename and ename == "pe":
                            continue
                        key, v = d.eng, d.val
                    elif d.kind == "d":
                        key, v = d.slot, d.dval
                    else:
                        key, v = "cc", d.dval
                    if seen.get(key, 0) >= v:
                        continue
                    seen[key] = v
                    e.wait_ge(sem[key], v)
                ins = o.fn(e)
                if o.kind == "c":
                    if o.target:
                        ins.then_inc(sem[ename], 1)
                elif o.kind == "d":
                    ins.then_inc(sem[o.slot], 16)
                else:
                    ins.then_inc(sem["cc"])
            if ename == "sp":
                for d in final:
                    e.wait_ge(sem[d.slot], d.dval)

        @block.tensor
        def _(e):
            run("pe", e)

        @block.scalar
        def _(e):
            run("act", e)

        @block.vector
        def _(e):
            run("dve", e)

        @block.gpsimd
        def _(e):
            run("pool", e)

        @block.sync
        def _(e):
            run("sp", e)


def build(kinds, wseq=None):
    nc = bass.Bass("TRN2", target_bir_lowering=False)
    k = K(nc)
    NL = len(kinds)
    offs, NA, NBk = layer_offsets(kinds)
    n_ret = sum(1 for x in kinds if x == 'ret')

    def dram(name, shape, dt, kind=None):
        if kind is None:
            return nc.dram_tensor(name, shape, dt)
        return nc.dram_tensor(name, shape, dt, kind=kind)

    xT_d = dram("xT", [8, 128, T], F32, "ExternalInput")
    yT_d = dram("yT", [8, 128, T], F32, "ExternalOutput")
    pT_d = dram("pT", [NL, 2, 128, T], F32, "ExternalInput")
    pos_d = dram("pos", [128, T], I32, "ExternalInput")
    wA_d = dram("wA", [NA, 128, 2048], F32, "ExternalInput")
    wB_d = dram("wB", [NBk, 128, 2816], F32, "ExternalInput")
    gains_d = dram("gains", [128, NL * 64], F32, "ExternalInput")
    gn_d = dram("gnb", [max(n_ret, 1) * RH, 128, 512], F32, "ExternalInput")
    cf_d = dram("cf", [128, 8], F32, "ExternalInput")
    rc_d = dram("rconst", [128, 4 * 128 + 4 + 4 + 64], F32, "ExternalInput")
    cb_d = dram("cbf", [128, 128 * 4 + 4 * 512], F32, "ExternalInput")

    cs_scr = dram("cs_scr", [2, 128, T], F32)
    qscr = [dram(f"qscr{h}", [128, T], BF16) for h in range(SH)]
    xs = [dram(f"xs{h}", [256, T], BF16) for h in range(SH)]
    xd = [dram(f"xd{h}", [512, T], BF16) for h in range(SH)]
    sxs = [dram(f"sxs{i}", [512, 512], F32) for i in range(2)]
    sxd = [dram(f"sxd{i}", [1024, 512], F32) for i in range(2)]
    dbuf = {}

    def DT(t, ap=None):
        key = id(t)
        if key not in dbuf:
            dbuf[key] = Buf()
        return TV(dbuf[key], ap if ap is not None else t.ap())

    io_buf = Buf()

    stack = contextlib.ExitStack()

    def sb(shape, dt, name=None, st=None):
        t = (st or stack).enter_context(nc.sbuf_tensor(shape, dt))
        return TV(Buf(), t.ap())

    def multi(shape, dt, n, st=None):
        return [sb(shape, dt, st=st) for _ in range(n)]

    with stack:
        hT = [[sb([128, 512], F32) for g in range(NG)] for dt in range(8)]
        gains = sb([128, NL * 64], F32)
        cf = sb([128, 8], F32)
        ident = sb([128, 128], BF16)
        ones = sb([128, 128], BF16)
        tri = sb([128, 128], BF16)
        negones = sb([128, 128], BF16)
        fscr = sb([128, 2], F32)
        NB_, LA_ = 5, 3
        wbf = multi([128, 2816], BF16, NB_)
        pbank = []
        pdbl = []
        for i in range(3):
            t = stack.enter_context(nc.psum_tensor([128, 1024], F32))
            pdbl.append(t.ap())
            pbank.append(TV(Buf(), t.ap()[:, 0:512]))
            pbank.append(TV(Buf(), t.ap()[:, 512:1024]))
        t = stack.enter_context(nc.psum_tensor([128, 512], F32))
        pbank.append(TV(Buf(), t.ap()))
        ptb_t = stack.enter_context(nc.psum_tensor([128, 1024], BF16))
        ptr = [TV(Buf(), ptb_t.ap()[:, i * 256:(i + 1) * 256]) for i in range(4)]
        pctr = {}

        def psum(ring=None):
            ring = tuple(ring or range(7))
            n = pctr.get(ring, 0)
            pctr[ring] = n + 1
            return pbank[ring[n % len(ring)]]

        ptc = [0]

        def ptrans():
            i = ptc[0] % 4
            ptc[0] += 1
            return ptr[i]

        def dma(out, in_, q="sp", nofence=False):
            return k.op(q, lambda e: e.dma_start(out=out.ap, in_=in_.ap), reads=[in_], writes=[out], kind="d",
                        nofence=nofence)

        def mm(out, lhsT, rhs, start, stop, extra_reads=()):
            return k.op("pe", lambda e: e.matmul(out.ap, lhsT.ap, rhs.ap, start=start, stop=stop),
                        reads=[lhsT, rhs] + ([] if start else [out]), writes=[out])

        def mmx(out, lhsT, rhs, start, stop):
            return k.op("pe", lambda e: e.matmul(out.ap, lhsT.ap, rhs.ap, start=start, stop=stop,
                                                 skip_group_check=True),
                        reads=[lhsT, rhs, out], writes=[out])

        def transpose(out, in_):
            return k.op("pe", lambda e: e.transpose(out.ap, in_.ap, ident.ap), reads=[in_, ident], writes=[out])

        def act(out, in_, func, bias=None, scale=None):
            rd = [in_]
            kw = {}
            if bias is not None:
                if isinstance(bias, TV):
                    rd.append(bias)
                    kw["bias"] = bias.ap
                else:
                    kw["bias"] = float(bias)
            if scale is not None:
                if isinstance(scale, TV):
                    rd.append(scale)
                    kw["scale"] = scale.ap
                else:
                    kw["scale"] = float(scale)
            return k.op("act", lambda e: e.activation(out=out.ap, in_=in_.ap, func=func, **kw), reads=rd,
                        writes=[out])

        def tt(out, in0, in1, op, eng="dve"):
            return k.op(eng, lambda e: e.tensor_tensor(out=out.ap, in0=in0.ap, in1=in1.ap, op=op),
                        reads=[in0, in1], writes=[out])

        def ts(out, in0, s1, s2, op0, op1=None, eng="dve"):
            rd = [in0]
            a1 = s1.ap if isinstance(s1, TV) else s1
            a2 = s2.ap if isinstance(s2, TV) else s2
            if isinstance(s1, TV):
                rd.append(s1)
            if isinstance(s2, TV):
                rd.append(s2)
            if op1 is None:
                return k.op(eng, lambda e: e.tensor_scalar(out=out.ap, in0=in0.ap, scalar1=a1, scalar2=None,
                                                           op0=op0), reads=rd, writes=[out])
            return k.op(eng, lambda e: e.tensor_scalar(out=out.ap, in0=in0.ap, scalar1=a1, scalar2=a2, op0=op0,
                                                       op1=op1), reads=rd, writes=[out])

        def stt(out, in0, s, in1, op0, op1):
            rd = [in0, in1]
            a = s.ap if isinstance(s, TV) else s
            if isinstance(s, TV):
                rd.append(s)
            return k.op("dve", lambda e: e.scalar_tensor_tensor(out=out.ap, in0=in0.ap, scalar=a, in1=in1.ap,
                                                                op0=op0, op1=op1), reads=rd, writes=[out])

        def cp(out, in_, eng="dve", nofence=False):
            return k.op(eng, lambda e: e.tensor_copy(out=out.ap, in_=in_.ap), reads=[in_], writes=[out],
                        nofence=nofence)

        def barrier():
            k.barrier(fscr[:, 0:1])

        wctr = [0]
        wreq = []
        wiss = [0]
        CAST_ENG = "act"

        def _wissue(i):
            l, name, c = (wseq if wseq is not None else wreq)[i]
            cls, base, KT, CW = offs[l][name]
            n = KT * CW
            src = wA_d if cls == 'A' else wB_d
            wb_ = wbf[i % NB_]
            src_tv = TV(io_buf, src.ap()[base + c])
            k.op("pool", lambda e: e.dma_start(out=wb_.ap[:, 0:n], in_=src_tv.ap, max_dma_last_dim=4096),
                 reads=[src_tv], writes=[wb_], kind="d", nofence=True)

        def wload(l, name, c):
            cls, base, KT, CW = offs[l][name]
            n = KT * CW
            i = wctr[0]
            wctr[0] += 1
            if wseq is None:
                wreq.append((l, name, c))
                _wissue(i)
            else:
                assert wseq[i] == (l, name, c)
                while wiss[0] <= min(i + LA_, len(wseq) - 1):
                    _wissue(wiss[0])
                    wiss[0] += 1
            wb_ = wbf[i % NB_]
            return TV(wb_.buf, wb_.ap[:, 0:n].rearrange("p (k c) -> p k c", k=KT))

        for dt in range(8):
            for g in range(NG):
                dma(hT[dt][g], TV(io_buf, xT_d.ap()[dt, :, g * 512:(g + 1) * 512]))
        dma(gains, TV(io_buf, gains_d.ap()))
        dma(cf, TV(io_buf, cf_d.ap()))
        with contextlib.ExitStack() as ph:
            cst = sb([128, 128 * 4 + 4 * 512], F32, st=ph)
            dma(cst, TV(io_buf, cb_d.ap()))
            cp(ident, cst[:, 0:128])
            cp(ones, cst[:, 128:256])
            cp(tri, cst[:, 256:384])
            cp(negones, cst[:, 384:512])
            if n_ret > 0:
                posi = sb([128, T], I32, st=ph)
                u = sb([128, T], F32, st=ph)
                ki = sb([128, T], I32, st=ph)
                kf = sb([128, T], F32, st=ph)
                fr = sb([128, T], F32, st=ph)
                cm = sb([128, T], F32, st=ph)
                dma(posi, TV(io_buf, pos_d.ap()))
                cp(u, posi)
                ts(u, u, cf[:, 2:3], None, ALU.mult)
                ts(u, u, 1.0 / (2.0 * math.pi), None, ALU.mult)
                for which in range(2):
                    if which == 0:
                        ts(fr, u, 0.25, None, ALU.add)
                    else:
                        cp(fr, u)
                    cp(ki, fr)
                    cp(kf, ki)
                    tt(fr, fr, kf, ALU.subtract)
                    ts(cm, fr, 0.5, None, ALU.is_gt)
                    tt(fr, fr, cm, ALU.subtract)
                    ts(cm, fr, -0.5, None, ALU.is_lt)
                    tt(fr, fr, cm, ALU.add)
                    act(kf, fr, AF.Sin, scale=2.0 * math.pi)
                    dma(DT(cs_scr, cs_scr.ap()[which]), kf)
            barrier()
        ident.buf.ro = ones.buf.ro = tri.buf.ro = negones.buf.ro = True
        gains.buf.ro = cf.buf.ro = True

        flag = cf[:, 0:1]
        negb = cf[:, 1:2]

        def gcol(l, n, dt):
            c = l * 64 + n * 8 + dt
            return gains[:, c:c + 1]

        def rstd_of(srcs, ph_tiles):
            sqr, rst = ph_tiles
            ps = psum()
            for dt in range(8):
                sq = sqr[dt % len(sqr)]
                act(sq, srcs[dt], AF.Square)
                mm(ps, ones, sq, dt == 0, dt == 7)
            r = rst[0]
            rst.append(rst.pop(0))
            act(r, ps, AF.Ln, bias=RMS_EPS, scale=1.0 / D)
            act(r, r, AF.Exp, scale=-0.5)
            return r

        def prenorm(l, n, groups, xn, ph_tiles):
            for gi, g in enumerate(groups):
                r = rstd_of([hT[dt][g] for dt in range(8)], ph_tiles)
                for dt in range(8):
                    stt(xn[dt][gi], hT[dt][g], gcol(l, n, dt), r, ALU.mult, ALU.mult)

        def postnorm(l, n, g, fsb, wgt, ph_tiles, tmp):
            r = rstd_of(fsb, ph_tiles)
            for dt in range(8):
                t_ = tmp[dt % len(tmp)]
                stt(t_, fsb[dt], gcol(l, n, dt), r, ALU.mult, ALU.mult)
                stt(hT[dt][g], t_, float(wgt), hT[dt][g], ALU.mult, ALU.add)

        def norm_tiles(ph):
            return (multi([128, 512], BF16, 4, st=ph), multi([128, 512], F32, 2, st=ph))

        def ffn(l, f):
            with contextlib.ExitStack() as ph:
                nt = norm_tiles(ph)
                sqr, rst = nt
                hh = [multi([128, 512], BF16, 2, st=ph) for _ in range(NFT)]
                fsb = [multi([128, 512], F32, 2, st=ph) for _ in range(8)]
                xn = [multi([128, 512], BF16, 2, st=ph) for _ in range(8)]
                sgt = multi([128, 512], F32, 2, st=ph)
                tmp = multi([128, 512], F32, 2, st=ph)
                RING = [0, 1, 2, 3, 4]
                NBANK = [pbank[5], pbank[6]]
                n_pre, n_post = 4 * f, 4 * f + 1

                def next_r():
                    r = rst[0]
                    rst.append(rst.pop(0))
                    return r

                def pre_sq(hf):
                    for gi in range(2):
                        for dt in range(8):
                            act(xn[dt][gi], hT[dt][2 * hf + gi], AF.Square)

                def pre_mm(hf):
                    for gi in range(2):
                        for dt in range(8):
                            mm(NBANK[gi], ones, xn[dt][gi], dt == 0, dt == 7)

                def pre_fin(hf):
                    for gi in range(2):
                        g = 2 * hf + gi
                        r = next_r()
                        act(r, NBANK[gi], AF.Ln, bias=RMS_EPS, scale=1.0 / D)
                        act(r, r, AF.Exp, scale=-0.5)
                        for dt in range(8):
                            stt(xn[dt][gi], hT[dt][g], gcol(l, n_pre, dt), r, ALU.mult, ALU.mult)

                def post_sq(dt):
                    for gi in range(2):
                        act(sqr[(2 * dt + gi) % len(sqr)], fsb[dt][gi], AF.Square)

                def post_mm(dt):
                    for gi in range(2):
                        mm(NBANK[gi], ones, sqr[(2 * dt + gi) % len(sqr)], dt == 0, dt == 7)

                def post_fin(hf):
                    for gi in range(2):
                        g = 2 * hf + gi
                        r = next_r()
                        act(r, NBANK[gi], AF.Ln, bias=RMS_EPS, scale=1.0 / D)
                        act(r, r, AF.Exp, scale=-0.5)
                        for dt in range(8):
                            t_ = tmp[dt % 2]
                            stt(t_, fsb[dt][gi], gcol(l, n_post, dt), r, ALU.mult, ALU.mult)
                            stt(hT[dt][g], t_, 0.5, hT[dt][g], ALU.mult, ALU.add)

                def gateup(hf, overlap_post):
                    for c in range(11):
                        if overlap_post and c < 8:
                            post_sq(c)
                        wg = wload(l, f"g{f}", c)
                        wu = wload(l, f"u{f}", c)
                        for sub in range(2):
                            ft = 2 * c + sub
                            for gi in range(2):
                                pg = psum(RING)
                                pu = psum(RING)
                                for kt in range(8):
                                    mm(pg, wg[:, kt, sub * 128:(sub + 1) * 128], xn[kt][gi], kt == 0, kt == 7)
                                for kt in range(8):
                                    mm(pu, wu[:, kt, sub * 128:(sub + 1) * 128], xn[kt][gi], kt == 0, kt == 7)
                                s_ = sgt[(ft * 2 + gi) % 2]
                                act(s_, pg, AF.Silu)
                                tt(hh[ft][gi], s_, pu, ALU.mult)
                        if overlap_post and c < 8:
                            post_mm(c)
                        if overlap_post and c == 8:
                            post_fin(hf - 1)

                def down(hf, overlap_pre):
                    for mt in range(8):
                        if overlap_pre and mt == 0:
                            pre_sq(hf + 1)
                        wd = wload(l, f"d{f}", mt)
                        for gi in range(2):
                            pf = psum(RING)
                            for ft in range(NFT):
                                mm(pf, wd[:, ft, :], hh[ft][gi], ft == 0, ft == NFT - 1)
                            act(fsb[mt][gi], pf, AF.Copy)
                        if overlap_pre and mt == 1:
                            pre_mm(hf + 1)
                        if overlap_pre and mt == 2:
                            pre_fin(hf + 1)

                pre_sq(0)
                pre_mm(0)
                pre_fin(0)
                gateup(0, False)
                down(0, True)
                gateup(1, True)
                down(1, False)
                for dt in range(8):
                    post_sq(dt)
                    post_mm(dt)
                post_fin(1)
                barrier()

        def ple(l):
            with contextlib.ExitStack() as ph:
                nt = norm_tiles(ph)
                xn = [multi([128, 512], BF16, 2, st=ph) for _ in range(8)]
                fsb = [multi([128, 512], F32, 2, st=ph) for _ in range(8)]
                sgt = multi([128, 512], F32, 2, st=ph)
                tmp = multi([128, 512], F32, 2, st=ph)
                pf32 = multi([128, 1024], F32, 2, st=ph)
                pb = multi([128, 1024], BF16, 2, st=ph)
                for hf in range(2):
                    groups = [2 * hf, 2 * hf + 1]
                    for kt in range(2):
                        dma(pf32[kt], TV(io_buf, pT_d.ap()[l, kt, :, hf * 1024:(hf + 1) * 1024]))
                        cp(pb[kt], pf32[kt], eng="pool")
                    prenorm(l, 6, groups, xn, nt)
                    for c in range(4):
                        wp = wload(l, "pp", 0)
                        wg = wload(l, "pg", c)
                        for sub in range(2):
                            mt = 2 * c + sub
                            for gi in range(2):
                                pg = psum()
                                pe_ = psum()
                                for kt in range(8):
                                    mm(pg, wg[:, kt, sub * 128:(sub + 1) * 128], xn[kt][gi], kt == 0, kt == 7)
                                for kt in range(2):
                                    mm(pe_, wp[:, kt, mt * 128:(mt + 1) * 128], pb[kt][:, gi * 512:(gi + 1) * 512],
                                       kt == 0, kt == 1)
                                s_ = sgt[(mt * 2 + gi) % 2]
                                act(s_, pg, AF.Sigmoid)
                                tt(fsb[mt][gi], s_, pe_, ALU.mult)
                    for gi in range(2):
                        postnorm(l, 7, groups[gi], [fsb[mt][gi] for mt in range(8)], 1.0, nt, tmp)
                barrier()

        def sb_mixer(l):
            with contextlib.ExitStack() as ph:
                nt = norm_tiles(ph)
                xn = [multi([128, 512], BF16, 2, st=ph) for _ in range(8)]
                stq = multi([128, 1024], BF16, 4, st=ph)
                vst = multi([128, 8, 128], BF16, 4, st=ph)
                sc = 0
                for hf in range(2):
                    groups = [2 * hf, 2 * hf + 1]
                    prenorm(l, 2, groups, xn, nt)
                    for qk in range(2):
                        for c in range(4):
                            w = wload(l, "min", qk * 4 + c)
                            for sub in range(2):
                                h = 2 * c + sub
                                st_ = stq[sc % 4]
                                sc += 1
                                for gi in range(2):
                                    ps = psum()
                                    for kt in range(8):
                                        mm(ps, w[:, kt, sub * 128:(sub + 1) * 128], xn[kt][gi], kt == 0, kt == 7)
                                    act(st_[:, gi * 512:(gi + 1) * 512], ps, AF.Copy,
                                        scale=(SDH ** -0.5 if qk == 0 else 1.0))
                                if qk == 0:
                                    dma(DT(qscr[h], qscr[h].ap()[:, hf * 1024:(hf + 1) * 1024]), st_)
                                else:
                                    dma(DT(xs[h], xs[h].ap()[0:128, hf * 1024:(hf + 1) * 1024]), st_)
                    for c in range(4):
                        w = wload(l, "min", 8 + c)
                        va = vst[(2 * c) % 4]
                        vb = vst[(2 * c + 1) % 4]
                        for b in range(8):
                            gi, bb = b // 4, b % 4
                            ps = psum()
                            for kt in range(8):
                                mm(ps[:, 0:256], xn[kt][gi][:, bb * 128:(bb + 1) * 128], w[:, kt, :], kt == 0, kt == 7)
                            act(va[:, b, :], ps[:, 0:128], AF.Copy)
                            act(vb[:, b, :], ps[:, 128:256], AF.Copy)
                        for sub, vv in ((0, va), (1, vb)):
                            h = 2 * c + sub
                            dma(DT(xs[h], xs[h].ap()[128:256, hf * 1024:(hf + 1) * 1024].rearrange(
                                "p (b d) -> p b d", b=8)), vv)
                barrier()
            for h in range(SH):
                s_, d_ = DT(xs[h]), DT(xd[h])
                k.op("pool", lambda e, s_=s_, d_=d_: e.collective_compute(
                    "AllGather", ALU.bypass, replica_groups=[[0, 1], [2, 3], [4, 5], [6, 7]],
                    ins=[s_.ap.opt()], outs=[d_.ap.opt()]), reads=[s_], writes=[d_], kind="cc")
            with contextlib.ExitStack() as ph:
                nt = norm_tiles(ph)
                oT = [multi([128, 512], BF16, NG, st=ph) for _ in range(SH)]
                qt = multi([128, T], BF16, 1, st=ph)
                kall = multi([128, 2 * T], BF16, 1, st=ph)
                vall = multi([128, 32, 128], BF16, 1, st=ph)
                et = multi([128, 1024], F32, 2, st=ph)
                spt = multi([128, 1024], BF16, 3, st=ph)
                at = multi([128, 1024], BF16, 2, st=ph)
                lsum = multi([128, 512], BF16, 6, st=ph)
                fsb = multi([128, 512], F32, 8, st=ph)
                tmp = [et[0][:, 0:512], et[1][:, 0:512]]
                maskb = sb([128, 4, 512], BF16, st=ph)
                for kl in range(4):
                    dma(et[kl % 2][:, 0:512], TV(io_buf, cb_d.ap()[:, 512 + kl * 512:512 + (kl + 1) * 512]))
                    cp(maskb[:, kl, :], et[kl % 2][:, 0:512])
                tc_ = 0
                pc_ = 0
                for h in range(SH):
                    q_ = qt[0]
                    K_ = kall[0]
                    V_ = vall[0]
                    dma(q_, DT(qscr[h]))
                    dma(K_[:, 0:T], DT(xd[h], xd[h].ap()[0:128, :]))
                    dma(K_[:, T:2 * T], DT(xs[h], xs[h].ap()[0:128, :]))
                    dma(V_[:, 0:16, :], DT(xd[h], xd[h].ap()[128:256, :].rearrange("p (b d) -> p b d", b=16)))
                    dma(V_[:, 16:32, :], DT(xs[h], xs[h].ap()[128:256, :].rearrange("p (b d) -> p b d", b=16)))
                    pairs = []
                    for qg in range(NG):
                        tiles = [(16 + kb, kb - 4 * qg if kb >= 4 * qg else None, False)
                                 for kb in range(4 * qg + 3, -1, -1)]
                        tiles += [(kb, None, True) for kb in range(15, -1, -1)]
                        tds = []
                        for ti, (kb, kl, prev) in enumerate(tiles):
                            tds.append(dict(kb=kb, kl=kl, prev=prev, first=ti == 0, last=ti == len(tiles) - 1,
                                            L=lsum[tc_ % 6], Ln=lsum[(tc_ + 1) % 6]))
                            tc_ += 1
                        for i in range(0, len(tds), 2):
                            assert tds[i]["prev"] == tds[i + 1]["prev"]
                            pairs.append(dict(qg=qg, t=tds[i:i + 2], po=pbank[4 + (h * NG + qg) % 2], k=pc_ % 2,
                                              e=et[pc_ % 2], s=spt[pc_ % 3], a=at[pc_ % 2],
                                              bias=(negb if tds[i]["prev"] else None)))
                            pc_ += 1

                    def pact(out, p, func, in_tv=None, bias=None):
                        kw = {}
                        rd = []
                        if in_tv is None:
                            in_ap = pdbl[p["k"]]
                            rd += [pbank[2 * p["k"]], pbank[2 * p["k"] + 1]]
                        else:
                            in_ap = in_tv.ap
                            rd.append(in_tv)
                        if isinstance(bias, TV):
                            kw["bias"] = bias.ap
                            rd.append(bias)
                        elif bias is not None:
                            kw["bias"] = float(bias)
                        k.op("act", lambda e: e.activation(out=out.ap, in_=in_ap, func=func, **kw), reads=rd,
                             writes=[out])

                    def stageA(p):
                        qv = q_[:, p["qg"] * 512:(p["qg"] + 1) * 512]
                        for j_, t in enumerate(p["t"]):
                            pz = pbank[2 * p["k"] + j_]
                            kv = K_[:, t["kb"] * 128:(t["kb"] + 1) * 128]
                            mm(pz, kv, qv, True, t["kl"] is None)
                            if t["kl"] is not None:
                                mm(pz, ident, maskb[:, t["kl"], :], False, True)
                        pact(p["e"], p, AF.Exp, bias=p["bias"])
                        pact(p["s"], p, AF.Ln, in_tv=p["e"], bias=1.0)
                        for j_, t in enumerate(p["t"]):
                            sh = p["s"][:, j_ * 512:(j_ + 1) * 512]
                            if not t["last"]:
                                if t["first"]:
                                    cp(t["Ln"], sh)
                                else:
                                    tt(t["Ln"], t["L"], sh, ALU.add)

                    def stageB1(p):
                        for j_, t in enumerate(p["t"]):
                            pz = pbank[2 * p["k"] + j_]
                            sh = p["s"][:, j_ * 512:(j_ + 1) * 512]
                            mmx(pz, tri, sh, False, t["first"])
                            if not t["first"]:
                                mmx(pz, negones, t["L"], False, True)
                        pact(p["a"], p, AF.Exp, bias=p["bias"])

                    def stageB2(p):
                        for j_, t in enumerate(p["t"]):
                            ah = p["a"][:, j_ * 512:(j_ + 1) * 512]
                            mm(p["po"], V_[:, t["kb"], :], ah, t["first"], t["last"])
                            if t["last"]:
                                cp(oT[h][p["qg"]], p["po"])

                    n_ = len(pairs)
                    for i in range(-2, n_):
                        if 0 <= i + 2 < n_:
                            stageA(pairs[i + 2])
                        if 0 <= i + 1 < n_:
                            stageB1(pairs[i + 1])
                        if i >= 0:
                            stageB2(pairs[i])
                for g in range(NG):
                    for c in range(4):
                        w = wload(l, "mout", c)
                        for sub in range(2):
                            mt = 2 * c + sub
                            ps = psum([2, 3, 4, 5, 6])
                            for hh_ in range(SH):
                                mm(ps, w[:, hh_, sub * 128:(sub + 1) * 128], oT[hh_][g], hh_ == 0, hh_ == SH - 1)
                            act(fsb[mt], ps, AF.Copy)
                    postnorm(l, 3, g, fsb, 1.0, nt, tmp)
                barrier()

        def ret_mixer(l, j):
            rcb = [None]
            mask_of = lambda h: rcb[0][:, h * 128:(h + 1) * 128]
            xi_of = lambda h: rcb[0][:, 512 + h:513 + h]
            zeta_of = lambda h: rcb[0][:, 516 + h:517 + h]
            zeta2_of = lambda h, c: rcb[0][:, 520 + h * 16 + c:521 + h * 16 + c]

            def load_rconst(ph):
                rcb[0] = sb([128, 4 * 128 + 4 + 4 + 64], F32, st=ph)
                dma(rcb[0], TV(io_buf, rc_d.ap()))

            def rope(dst, p1, p2, cos_, sin_, tm):
                tt(tm[0], p1, cos_, ALU.mult)
                tt(tm[1], p2, sin_, ALU.mult)
                tt(dst[0], tm[0], tm[1], ALU.subtract)
                tt(tm[2], p1, sin_, ALU.mult)
                tt(tm[3], p2, cos_, ALU.mult)
                tt(dst[1], tm[2], tm[3], ALU.add)

            with contextlib.ExitStack() as ph:
                nt = norm_tiles(ph)
                load_rconst(ph)
                xn = [multi([128, 512], BF16, NG, st=ph) for _ in range(8)]
                cs = multi([128, 512], F32, 4, st=ph)
                krT = multi([128, T], BF16, 2, st=ph)
                vh = sb([128, 16, 512], BF16, st=ph)
                tm = multi([128, 512], F32, 4, st=ph)
                kz = multi([128, 256], BF16, 2, st=ph)
                sev = multi([128, 512], F32, 2, st=ph)
                prenorm(l, 2, [0, 1, 2, 3], xn, nt)
                for h in range(RH):
                    w = wload(l, "min", 4 + h)
                    for g in range(NG):
                        p1, p2 = psum([2, 3, 4, 5, 6]), psum([2, 3, 4, 5, 6])
                        for kt in range(8):
                            mm(p1, w[:, kt, 0:128], xn[kt][g], kt == 0, kt == 7)
                        for kt in range(8):
                            mm(p2, w[:, kt, 128:256], xn[kt][g], kt == 0, kt == 7)
                        sl = slice(g * 512, (g + 1) * 512)
                        c0, c1 = cs[(g % 2) * 2], cs[(g % 2) * 2 + 1]
                        dma(c0, DT(cs_scr, cs_scr.ap()[0, :, sl]))
                        dma(c1, DT(cs_scr, cs_scr.ap()[1, :, sl]))
                        rope([krT[0][:, sl], krT[1][:, sl]], p1, p2, c0, c1, tm)
                    for cc_ in range(2):
                        w = wload(l, "min", 8 + 2 * h + cc_)
                        for b in range(16):
                            g, bb = b // 4, b % 4
                            ps = psum([2, 3, 4, 5, 6])
                            for kt in range(8):
                                mm(ps[:, 0:256], xn[kt][g][:, bb * 128:(bb + 1) * 128], w[:, kt, :], kt == 0, kt == 7)
                            act(vh[:, b, cc_ * 256:(cc_ + 1) * 256], ps[:, 0:256], AF.Copy)
                    pS = [pbank[0], pbank[1]]
                    for b in range(16):
                        pt = ptrans()
                        for a in range(2):
                            transpose(pt[:, a * 128:(a + 1) * 128], krT[a][:, b * 128:(b + 1) * 128])
                        kz_ = kz[b % 2]
                        act(kz_, pt, AF.Copy, scale=zeta2_of(h, b))
                        for a in range(2):
                            mm(pS[a], kz_[:, a * 128:(a + 1) * 128], vh[:, b, :], b == 0, b == 15)
                    for a in range(2):
                        i = h * 2 + a
                        cp(sev[a], pS[a])
                        dma(DT(sxs[i // 4], sxs[i // 4].ap()[(i % 4) * 128:(i % 4 + 1) * 128, :]), sev[a])
                barrier()
            for i in range(2):
                s_, d_ = DT(sxs[i]), DT(sxd[i])
                k.op("pool", lambda e, s_=s_, d_=d_: e.collective_compute(
                    "AllGather", ALU.bypass, replica_groups=[[0, 1], [2, 3], [4, 5], [6, 7]],
                    ins=[s_.ap.opt()], outs=[d_.ap.opt()]), reads=[s_], writes=[d_], kind="cc")
            with contextlib.ExitStack() as ph:
                nt = norm_tiles(ph)
                load_rconst(ph)
                ogT = multi([128, 512], BF16, 16, st=ph)
                xn = [[ogT[8 + dt]] for dt in range(8)]
                cs = multi([128, 512], F32, 2, st=ph)
                LN = 2
                qrT = [multi([128, 512], BF16, 2, st=ph) for _ in range(LN)]
                krT = [multi([128, 512], BF16, 2, st=ph) for _ in range(LN)]
                vh = [sb([128, 4, 512], BF16, st=ph) for _ in range(LN)]
                sgh = [sb([128, 4, 512], BF16, st=ph) for _ in range(LN)]
                gnt = multi([128, 512], F32, 2, st=ph)
                S = [multi([128, 512], F32, 2, st=ph) for _ in range(RH)]
                Sbf = [multi([128, 512], BF16, 2, st=ph) for _ in range(LN)]
                big = multi([128, 512], F32, 10, st=ph)
                tm = big[0:4]
                osb = [big[4:7], big[7:10]]
                scm = [multi([128, 128], BF16, 2, st=ph) for _ in range(LN)]
                kz = [multi([128, 256], BF16, 2, st=ph) for _ in range(LN)]
                og = [multi([128, 512], BF16, 2, st=ph) for _ in range(LN)]
                st6 = [multi([128, 6], F32, 2, st=ph) for _ in range(LN)]
                mv = [multi([128, 2], F32, 2, st=ph) for _ in range(LN)]
                rs = [multi([128, 1], F32, 2, st=ph) for _ in range(LN)]
                fsb = big[0:8]
                tmp = gnt
                ring = [2, 3, 4, 5, 6]

                def project(g, h, ln):
                    for qk, dst in ((0, qrT[ln]), (1, krT[ln])):
                        w = wload(l, "min", qk * 4 + h)
                        p1, p2 = psum(ring), psum(ring)
                        for kt in range(8):
                            mm(p1, w[:, kt, 0:128], xn[kt][0], kt == 0, kt == 7)
                        for kt in range(8):
                            mm(p2, w[:, kt, 128:256], xn[kt][0], kt == 0, kt == 7)
                        rope(dst, p1, p2, cs[0], cs[1], tm)
                    gn_ = gnt[ln]
                    dma(gn_, TV(io_buf, gn_d.ap()[j * RH + h]))
                    for vg, dstt in ((0, vh[ln]), (1, sgh[ln])):
                        for cc_ in range(2):
                            w = wload(l, "min", 8 + vg * 8 + 2 * h + cc_)
                            for b in range(4):
                                ps = psum(ring)
                                for kt in range(8):
                                    mm(ps[:, 0:256], xn[kt][0][:, b * 128:(b + 1) * 128], w[:, kt, :],
                                       kt == 0, kt == 7)
                                if vg == 0:
                                    act(dstt[:, b, cc_ * 256:(cc_ + 1) * 256], ps[:, 0:256], AF.Copy)
                                else:
                                    t_ = tm[(b + cc_) % 4]
                                    act(t_[:, 0:256], ps[:, 0:256], AF.Silu)
                                    tt(dstt[:, b, cc_ * 256:(cc_ + 1) * 256], t_[:, 0:256],
                                       gn_[:, cc_ * 256:(cc_ + 1) * 256], ALU.mult)
                    if g == 0:
                        for a in range(2):
                            i = h * 2 + a
                            dma(tm[a], DT(sxd[i // 4], sxd[i // 4].ap()[(i % 4) * 128:(i % 4 + 1) * 128, :]))
                            ts(S[h][a], tm[a], flag, None, ALU.mult)
                    for a in range(2):
                        act(Sbf[ln][a], S[h][a], AF.Copy)

                def lane_stages(h, ln):
                    po = pbank[ln]

                    def m1(c):
                        bs = slice(c * 128, (c + 1) * 128)
                        psc = psum(ring)
                        for a in range(2):
                            mm(psc[:, 0:128], krT[ln][a][:, bs], qrT[ln][a][:, bs], a == 0, a == 1)
                        tt(scm[ln][c % 2], psc[:, 0:128], mask_of(h), ALU.mult)

                    def m3(c):
                        bs = slice(c * 128, (c + 1) * 128)
                        mm(po, scm[ln][c % 2], vh[ln][:, c, :], True, False)
                        for a in range(2):
                            mm(po, qrT[ln][a][:, bs], Sbf[ln][a], False, a == 1)
                        pt = ptrans()
                        for a in range(2):
                            transpose(pt[:, a * 128:(a + 1) * 128], krT[ln][a][:, bs])
                        act(kz[ln][c % 2], pt, AF.Copy, scale=zeta_of(h))
                        act(osb[ln][c % 3], po, AF.Copy, scale=xi_of(h))

                    def m5(c):
                        kz_ = kz[ln][c % 2]
                        pS = [psum(ring), psum(ring)]
                        for a in range(2):
                            mm(pS[a], kz_[:, a * 128:(a + 1) * 128], vh[ln][:, c, :], True, True)
                        for a in range(2):
                            stt(S[h][a], S[h][a], float(GAMMA[h] ** 128), pS[a], ALU.mult, ALU.add)

                    def m7(c):
                        for a in range(2):
                            act(Sbf[ln][a], S[h][a], AF.Copy)

                    def n1(c):
                        o_, s6, mv_ = osb[ln][c % 3], st6[ln][c % 2], mv[ln][c % 2]
                        k.op("dve", lambda e: e.bn_stats(out=s6.ap, in_=o_.ap), reads=[o_], writes=[s6])
                        k.op("dve", lambda e: e.bn_aggr(out=mv_.ap, in_=s6.ap), reads=[s6], writes=[mv_])

                    def n2(c):
                        mv_, rs_ = mv[ln][c % 2], rs[ln][c % 2]
                        act(rs_, mv_[:, 1:2], AF.Ln, bias=GN_EPS)
                        act(rs_, rs_, AF.Exp, scale=-0.5)

                    def n3(c):
                        o_, mv_, rs_, og_ = osb[ln][c % 3], mv[ln][c % 2], rs[ln][c % 2], og[ln][c % 2]
                        ts(o_, o_, mv_[:, 0:1], rs_, ALU.subtract, ALU.mult)
                        tt(og_, o_, sgh[ln][:, c, :], ALU.mult)

                    def n4(c):
                        bs = slice(c * 128, (c + 1) * 128)
                        og_ = og[ln][c % 2]
                        for half in range(2):
                            pt2 = ptrans()
                            for q2 in range(2):
                                q4 = half * 2 + q2
                                transpose(pt2[:, q2 * 128:(q2 + 1) * 128], og_[:, q4 * 128:(q4 + 1) * 128])
                            for q2 in range(2):
                                q4 = half * 2 + q2
                                cp(ogT[h * 4 + q4][:, bs], pt2[:, q2 * 128:(q2 + 1) * 128])

                    st = []

                    def add(fn, c):
                        if 0 <= c < 4:
                            st.append(lambda fn=fn, c=c: fn(c))

                    for c in range(6):
                        add(m1, c)
                        add(m3, c)
                        add(n1, c - 1)
                        add(m5, c)
                        add(n2, c - 1)
                        add(n4, c - 2)
                        add(m7, c)
                        add(n3, c - 1)
                    return st

                for g in range(NG):
                    sl = slice(g * 512, (g + 1) * 512)
                    dma(cs[0], DT(cs_scr, cs_scr.ap()[0, :, sl]))
                    dma(cs[1], DT(cs_scr, cs_scr.ap()[1, :, sl]))
                    prenorm(l, 2, [g], xn, nt)
                    for pair in range(2):
                        hs = (2 * pair, 2 * pair + 1)
                        for ln, h in enumerate(hs):
                            project(g, h, ln)
                        lists = [lane_stages(h, ln) for ln, h in enumerate(hs)]
                        if ZIP_LANES:
                            for i in range(max(len(x) for x in lists)):
                                for lst in lists:
                                    if i < len(lst):
                                        lst[i]()
                        else:
                            for lst in lists:
                                for fn_ in lst:
                                    fn_()
                    for mt in range(8):
                        w = wload(l, "mout", mt)
                        ps = psum(ring)
                        for kt in range(16):
                            mm(ps, w[:, kt, :], ogT[kt], kt == 0, kt == 15)
                        act(fsb[mt], ps, AF.Copy)
                    postnorm(l, 3, g, fsb, 1.0, nt, tmp)
                barrier()

        jr = 0
        for l, kind in enumerate(kinds):
            ffn(l, 0)
            if kind == 'ret':
                ret_mixer(l, jr)
                jr += 1
            else:
                sb_mixer(l)
            ffn(l, 1)
            ple(l)
        for dt in range(8):
            for g in range(NG):
                o = dma(TV(Buf(), yT_d.ap()[dt, :, g * 512:(g + 1) * 512]), hT[dt][g])
                k.out_dmas.append(o)
        k.emit(stack)
    return nc, wreq


_NC_CACHE = {}


def _get_nc(kinds):
    key = tuple(kinds)
    if key not in _NC_CACHE:
        _, seq = build(list(kinds))
        _NC_CACHE[key] = build(list(kinds), wseq=seq)[0]
    return _NC_CACHE[key]


def _const_inputs():
    f32 = np.float32
    rc = np.zeros((128, 4 * 128 + 4 + 4 + 64), f32)
    jj = np.arange(128, dtype=np.float64)
    for h in range(RH):
        lg = math.log1p(-2.0 ** (-5.0 - h))
        m = np.zeros((128, 128), np.float64)
        for j in range(128):
            m[j, j:] = (RDK ** -0.5) * math.exp(-(j + 1) * lg)
        rc[:, h * 128:(h + 1) * 128] = m
        rc[:, 512 + h] = np.exp((jj + 1.0) * lg)
        rc[:, 516 + h] = (RDK ** -0.5) * np.exp((127.0 - jj) * lg)
        for c in range(16):
            rc[:, 520 + h * 16 + c] = (RDK ** -0.5) * np.exp((2047.0 - (c * 128 + jj)) * lg)
    cb = np.zeros((128, 128 * 4 + 4 * 512), f32)
    cb[:, 0:128] = np.eye(128)
    cb[:, 128:256] = 1.0
    s = np.arange(128)
    cb[:, 256:384] = -(s[:, None] >= s[None, :]).astype(f32)
    cb[:, 384:512] = -1.0
    t = np.arange(512)
    for kl in range(4):
        ok = (kl * 128 + s[:, None]) < t[None, :]
        cb[:, 512 + kl * 512:512 + (kl + 1) * 512] = np.where(ok, 0.0, NEG)
    return rc, cb


def _prep_shared(kinds, lids, inp):
    offs, NA, NBk = layer_offsets(kinds)
    wA = np.zeros((NA, 128, 2048), np.float32)
    wB = np.zeros((max(NBk, 1), 128, 2816), np.float32)
    gains = np.zeros((128, len(kinds) * 64), np.float32)
    n_ret = sum(1 for x in kinds if x == 'ret')
    gnb = np.zeros((max(n_ret, 1) * RH, 128, 512), np.float32)
    jr = 0
    for li, (kind, L) in enumerate(zip(kinds, lids)):
        o = offs[li]

        def put(name, W):
            cls, base, KT, CW = o[name]
            pk = pack_w(np.asarray(W, np.float32), KT, CW)
            (wA if cls == 'A' else wB)[base:base + pk.shape[0]] = pk
        for f in range(2):
            put(f"g{f}", inp["ffn_w_gate"][L, f])
            put(f"u{f}", inp["ffn_w_up"][L, f])
            put(f"d{f}", inp["ffn_w_down"][L, f])
        j = L // 2
        if kind == 'ret':
            put("min", inp["ret_w_in"][j])
            put("mout", inp["ret_w_out"][j])
            gg = np.asarray(inp["ret_gn_gain"][j], np.float32).reshape(RH, 1, 512)
            gnb[jr * RH:(jr + 1) * RH] = np.broadcast_to(gg, (RH, 128, 512))
            jr += 1
        else:
            put("min", inp["sb_w_in"][j])
            put("mout", inp["sb_w_out"][j])
        put("pg", inp["ple_w_gate"][L])
        put("pp", inp["ple_w_proj"][L])
        g = np.asarray(inp["norm_gains"][L], np.float32)
        gains[:, li * 64:(li + 1) * 64] = g.reshape(8, 8, 128).transpose(2, 0, 1).reshape(128, 64)
    return wA, wB, gains, gnb


def _run(kinds, lids, inp, hT_cores):
    nc = _get_nc(kinds)
    wA, wB, gains, gnb = _prep_shared(kinds, lids, inp)
    rc, cb = _const_inputs()
    p = np.asarray(inp["p"], np.float32)
    pos = np.asarray(inp["positions"]).astype(np.int32)
    inv = (10000.0 ** (-np.arange(128, dtype=np.float32) / np.float32(128))).astype(np.float32)
    in_maps = []
    for c in range(8):
        b, hf = c // 2, c % 2
        tok = slice(hf * T, (hf + 1) * T)
        pT = np.stack([np.ascontiguousarray(p[L, b, tok].T).reshape(2, 128, T) for L in lids])
        cf = np.zeros((128, 8), np.float32)
        cf[:, 0] = float(hf)
        cf[:, 1] = 0.0 if hf == 1 else NEG
        cf[:, 2] = inv
        in_maps.append({
            "xT": hT_cores[c], "pT": pT,
            "pos": np.ascontiguousarray(np.broadcast_to(pos[b, tok][None, :], (128, T))),
            "wA": wA, "wB": wB, "gains": gains, "gnb": gnb, "cf": cf, "rconst": rc, "cbf": cb,
        })
    res = run_bass_kernel_spmd(nc, in_maps, core_ids=list(range(8)), **RUN_KW)
    return [np.asarray(res.results[c]["yT"]) for c in range(8)]


FUSED = True
RUN_KW = {}
ZIP_LANES = False


def kernel(**inp):
    x = np.asarray(inp["x"], np.float32)
    hT = []
    for c in range(8):
        b, hf = c // 2, c % 2
        hT.append(np.ascontiguousarray(x[b, hf * T:(hf + 1) * T].T).reshape(8, 128, T))
    all_kinds = ['ret' if i % 2 == 0 else 'sb' for i in range(DEPTH)]
    if FUSED:
        hT = _run(all_kinds, list(range(DEPTH)), inp, hT)
    else:
        for L in range(DEPTH):
            hT = _run([all_kinds[L]], [L], inp, hT)
    out = np.zeros((BATCH, SEQ, D), np.float32)
    for c in range(8):
        b, hf = c // 2, c % 2
        out[b, hf * T:(hf + 1) * T] = hT[c].reshape(D, T).T
    return out
```

```python
import contextlib
import math
import numpy as np
import concourse.bass as bass
import concourse.mybir as mybir
from concourse.bass_utils import run_bass_kernel_spmd

F32, BF16, I32 = mybir.dt.float32, mybir.dt.bfloat16, mybir.dt.int32
AF = mybir.ActivationFunctionType
ALU = mybir.AluOpType

D = 1024
SEQ = 4096
BATCH = 4
DEPTH = 4
T = 2048
NG = 4
DFF = 2816
NFT = 22
PLE = 256
RMS_EPS = 1e-6
GN_EPS = 1e-5
RH, RDK, RDV = 4, 256, 512
SH, SDH = 8, 128
NEG = -30000.0
GAMMA = [1.0 - 2.0 ** (-5.0 - h) for h in range(RH)]
OWN = ((0, 3, 4, 7), (1, 2, 5, 6))
SB_PRIORS = (
    ((0, 0, 'A'),),
    ((1, 1, 'B'), (1, 0, None), (0, 0, None)),
    ((0, 2, 'A'), (0, 1, None), (1, 1, None), (1, 0, None), (0, 0, None)),
    ((1, 3, 'B'), (1, 2, None), (0, 2, None), (0, 1, None), (1, 1, None), (1, 0, None), (0, 0, None)),
)


def layer_plan(kind):
    pl = []
    for f in range(2):
        pl += [(f"g{f}", 'A', 8, 256, 11), (f"u{f}", 'A', 8, 256, 11), (f"d{f}", 'B', 22, 128, 8)]
    if kind == 'ret':
        pl += [("min", 'A', 8, 256, 24), ("mout", 'A', 16, 128, 8)]
    else:
        pl += [("min", 'A', 8, 256, 12), ("mout", 'A', 8, 256, 4)]
    pl += [("pg", 'A', 8, 256, 4), ("pp", 'A', 2, 1024, 1)]
    return pl


def layer_offsets(kinds):
    na = nb = 0
    out = []
    for kind in kinds:
        d = {}
        for name, cls, kt, cw, n in layer_plan(kind):
            if cls == 'A':
                d[name] = (cls, na, kt, cw)
                na += n
            else:
                d[name] = (cls, nb, kt, cw)
                nb += n
        out.append(d)
    return out, na, nb


def pack_w(W, KT, CW):
    K, N = W.shape
    assert K == KT * 128 and N % CW == 0
    return np.ascontiguousarray(
        W.reshape(KT, 128, N // CW, CW).transpose(2, 1, 0, 3)).reshape(N // CW, 128, KT * CW)


class Buf:
    __slots__ = ("lw", "rd", "rdd", "ro")

    def __init__(self):
        self.lw = None
        self.rd = {}
        self.rdd = []
        self.ro = False


class TV:
    __slots__ = ("buf", "ap")

    def __init__(self, buf, ap):
        self.buf = buf
        self.ap = ap

    def __getitem__(self, idx):
        return TV(self.buf, self.ap[idx])


class Op:
    __slots__ = ("eng", "fn", "deps", "kind", "target", "val", "slot", "dval")

    def __init__(self, eng, fn, deps, kind):
        self.eng = eng
        self.fn = fn
        self.deps = deps
        self.kind = kind
        self.target = False
        self.val = 0
        self.slot = None
        self.dval = 0


ENGS = ("pe", "act", "dve", "pool", "sp")
NSLOT = {"sp": 24, "act": 8, "pool": 8}


class K:
    def __init__(self, nc):
        self.nc = nc
        self.streams = {e: [] for e in ENGS}
        self.fence = None
        self.dma_since_fence = []
        self.slot_ctr = {q: 0 for q in NSLOT}
        self.slot_last = {}
        self.slot_cnt = {}
        self.cc_cnt = 0
        self.out_dmas = []

    def op(self, eng, fn, reads=(), writes=(), kind="c", nofence=False):
        deps = set()
        for r in reads:
            if r.buf.lw is not None:
                deps.add(r.buf.lw)
        for w in writes:
            b = w.buf
            if b.lw is not None:
                deps.add(b.lw)
            deps.update(b.rd.values())
            deps.update(b.rdd)
        if self.fence is not None and not nofence:
            deps.add(self.fence)
        o = Op(eng, fn, deps, kind)
        if kind == "d":
            q = eng
            s = self.slot_ctr[q] % NSLOT[q]
            self.slot_ctr[q] += 1
            key = (q, s)
            if key in self.slot_last:
                deps.add(self.slot_last[key])
            self.slot_last[key] = o
            self.slot_cnt[key] = self.slot_cnt.get(key, 0) + 1
            o.slot = key
            o.dval = 16 * self.slot_cnt[key]
            self.dma_since_fence.append(o)
        elif kind == "cc":
            self.cc_cnt += 1
            o.dval = self.cc_cnt
            self.dma_since_fence.append(o)
        for r in reads:
            b = r.buf
            if b.ro:
                continue
            if kind == "c":
                b.rd[eng] = o
            else:
                b.rdd.append(o)
        for w in writes:
            b = w.buf
            b.lw = o
            b.rd = {}
            b.rdd = []
        deps.discard(o)
        self.streams[eng].append(o)
        return o

    def barrier(self, scratch):
        deps = set(self.dma_since_fence)
        for e in ENGS:
            for o in reversed(self.streams[e]):
                if o.kind == "c":
                    deps.add(o)
                    break
        if self.fence is not None:
            deps.add(self.fence)
        o = Op("dve", lambda e: e.memset(scratch.ap, 0.0), deps, "c")
        self.streams["dve"].append(o)
        self.fence = o
        self.dma_since_fence = []

    def emit(self, stack):
        nc = self.nc
        for e in ENGS:
            for o in self.streams[e]:
                for d in o.deps:
                    if d.kind == "c":
                        d.target = True
        sem = {}
        for e in ENGS:
            sem[e] = stack.enter_context(nc.semaphore("s_" + e))
            n = 0
            for o in self.streams[e]:
                if o.kind == "c" and o.target:
                    n += 1
                    o.val = n
        for q, ns in NSLOT.items():
            for s in range(ns):
                sem[(q, s)] = stack.enter_context(nc.semaphore(f"d_{q}{s}"))
        sem["cc"] = stack.enter_context(nc.semaphore("s_cc"))
        final = list(self.out_dmas)
        block = stack.enter_context(nc.Block())

        def run(ename, e):
            seen = {}
            for o in self.streams[ename]:
                for d in o.deps:
                    if d.kind == "c":
                        if d.eng == ename and ename == "pe":
                            continue
                        key, v = d.eng, d.val
                    elif d.kind == "d":
                        key, v = d.slot, d.dval
                    else:
                        key, v = "cc", d.dval
                    if seen.get(key, 0) >= v:
                        continue
                    seen[key] = v
                    e.wait_ge(sem[key], v)
                ins = o.fn(e)
                if o.kind == "c":
                    if o.target:
                        ins.then_inc(sem[ename], 1)
                elif o.kind == "d":
                    ins.then_inc(sem[o.slot], 16)
                else:
                    ins.then_inc(sem["cc"])
            if ename == "sp":
                for d in final:
                    e.wait_ge(sem[d.slot], d.dval)

        @block.tensor
        def _(e):
            run("pe", e)

        @block.scalar
        def _(e):
            run("act", e)

        @block.vector
        def _(e):
            run("dve", e)

        @block.gpsimd
        def _(e):
            run("pool", e)

        @block.sync
        def _(e):
            run("sp", e)


def build(kinds, wseq=None):
    nc = bass.Bass("TRN2", target_bir_lowering=False)
    k = K(nc)
    NL = len(kinds)
    offs, NA, NBk = layer_offsets(kinds)
    n_ret = sum(1 for x in kinds if x == 'ret')

    def dram(name, shape, dt, kind=None):
        if kind is None:
            return nc.dram_tensor(name, shape, dt)
        return nc.dram_tensor(name, shape, dt, kind=kind)

    xT_d = dram("xT", [8, 128, T], F32, "ExternalInput")
    yT_d = dram("yT", [8, 128, T], F32, "ExternalOutput")
    pT_d = dram("pT", [NL, 2, 128, T], F32, "ExternalInput")
    pos_d = dram("pos", [128, T], I32, "ExternalInput")
    wA_d = dram("wA", [NA, 128, 2048], F32, "ExternalInput")
    wB_d = dram("wB", [NBk, 128, 2816], F32, "ExternalInput")
    gains_d = dram("gains", [128, NL * 64], F32, "ExternalInput")
    gn_d = dram("gnb", [max(n_ret, 1) * RH, 128, 512], F32, "ExternalInput")
    cf_d = dram("cf", [128, 160], F32, "ExternalInput")
    rc_d = dram("rconst", [128, 4 * 128 + 4 + 4 + 64], F32, "ExternalInput")
    cb_d = dram("cbf", [128, 128 * 4 + 4 * 512], F32, "ExternalInput")

    cs_scr = dram("cs_scr", [2, 128, T], F32)
    qscr = [dram(f"qscr{h}", [128, T], BF16) for h in range(SH)]
    xs = [dram(f"xs{h}", [256, T], BF16) for h in range(SH)]
    xd = [dram(f"xd{h}", [512, T], BF16) for h in range(SH)]
    sxs = [dram(f"sxs{i}", [512, 512], F32) for i in range(4)]
    sxd = [dram(f"sxd{i}", [1024, 512], F32) for i in range(4)]
    dbuf = {}

    def DT(t, ap=None):
        key = id(t)
        if key not in dbuf:
            dbuf[key] = Buf()
        return TV(dbuf[key], ap if ap is not None else t.ap())

    io_buf = Buf()

    stack = contextlib.ExitStack()

    def sb(shape, dt, name=None, st=None):
        t = (st or stack).enter_context(nc.sbuf_tensor(shape, dt))
        return TV(Buf(), t.ap())

    def multi(shape, dt, n, st=None):
        return [sb(shape, dt, st=st) for _ in range(n)]

    with stack:
        hT = [[sb([128, 512], F32) for g in range(NG)] for dt in range(8)]
        gains = sb([128, NL * 64], F32)
        cf = sb([128, 160], F32)
        ident = sb([128, 128], BF16)
        ones = sb([128, 128], BF16)
        tri = sb([128, 128], BF16)
        negones = sb([128, 128], BF16)
        fscr = sb([128, 2], F32)
        NB_, LA_ = 5, 3
        wbf = multi([128, 2816], BF16, NB_)
        pbank = []
        for i in range(7):
            t = stack.enter_context(nc.psum_tensor([128, 512], F32))
            pbank.append(TV(Buf(), t.ap()))
        ptb_t = stack.enter_context(nc.psum_tensor([128, 1024], BF16))
        ptr = [TV(Buf(), ptb_t.ap()[:, i * 256:(i + 1) * 256]) for i in range(4)]
        pctr = {}

        def psum(ring=None):
            ring = tuple(ring or range(7))
            n = pctr.get(ring, 0)
            pctr[ring] = n + 1
            return pbank[ring[n % len(ring)]]

        ptc = [0]

        def ptrans():
            i = ptc[0] % 4
            ptc[0] += 1
            return ptr[i]

        def dma(out, in_, q="sp", nofence=False):
            return k.op(q, lambda e: e.dma_start(out=out.ap, in_=in_.ap), reads=[in_], writes=[out], kind="d",
                        nofence=nofence)

        def mm(out, lhsT, rhs, start, stop, extra_reads=()):
            return k.op("pe", lambda e: e.matmul(out.ap, lhsT.ap, rhs.ap, start=start, stop=stop),
                        reads=[lhsT, rhs] + ([] if start else [out]), writes=[out])

        def mmx(out, lhsT, rhs, start, stop):
            return k.op("pe", lambda e: e.matmul(out.ap, lhsT.ap, rhs.ap, start=start, stop=stop,
                                                 skip_group_check=True),
                        reads=[lhsT, rhs, out], writes=[out])

        def transpose(out, in_):
            return k.op("pe", lambda e: e.transpose(out.ap, in_.ap, ident.ap), reads=[in_, ident], writes=[out])

        def act(out, in_, func, bias=None, scale=None):
            rd = [in_]
            kw = {}
            if bias is not None:
                if isinstance(bias, TV):
                    rd.append(bias)
                    kw["bias"] = bias.ap
                else:
                    kw["bias"] = float(bias)
            if scale is not None:
                if isinstance(scale, TV):
                    rd.append(scale)
                    kw["scale"] = scale.ap
                else:
                    kw["scale"] = float(scale)
            return k.op("act", lambda e: e.activation(out=out.ap, in_=in_.ap, func=func, **kw), reads=rd,
                        writes=[out])

        def tt(out, in0, in1, op, eng="dve"):
            return k.op(eng, lambda e: e.tensor_tensor(out=out.ap, in0=in0.ap, in1=in1.ap, op=op),
                        reads=[in0, in1], writes=[out])

        def ts(out, in0, s1, s2, op0, op1=None, eng="dve"):
            rd = [in0]
            a1 = s1.ap if isinstance(s1, TV) else s1
            a2 = s2.ap if isinstance(s2, TV) else s2
            if isinstance(s1, TV):
                rd.append(s1)
            if isinstance(s2, TV):
                rd.append(s2)
            if op1 is None:
                return k.op(eng, lambda e: e.tensor_scalar(out=out.ap, in0=in0.ap, scalar1=a1, scalar2=None,
                                                           op0=op0), reads=rd, writes=[out])
            return k.op(eng, lambda e: e.tensor_scalar(out=out.ap, in0=in0.ap, scalar1=a1, scalar2=a2, op0=op0,
                                                       op1=op1), reads=rd, writes=[out])

        def stt(out, in0, s, in1, op0, op1):
            rd = [in0, in1]
            a = s.ap if isinstance(s, TV) else s
            if isinstance(s, TV):
                rd.append(s)
            return k.op("dve", lambda e: e.scalar_tensor_tensor(out=out.ap, in0=in0.ap, scalar=a, in1=in1.ap,
                                                                op0=op0, op1=op1), reads=rd, writes=[out])

        def cp(out, in_, eng="dve", nofence=False):
            return k.op(eng, lambda e: e.tensor_copy(out=out.ap, in_=in_.ap), reads=[in_], writes=[out],
                        nofence=nofence)

        def barrier():
            k.barrier(fscr[:, 0:1])

        wctr = [0]
        wreq = []
        wiss = [0]
        CAST_ENG = "act"

        def _wissue(i):
            l, name, c = (wseq if wseq is not None else wreq)[i]
            cls, base, KT, CW = offs[l][name]
            n = KT * CW
            src = wA_d if cls == 'A' else wB_d
            wb_ = wbf[i % NB_]
            src_tv = TV(io_buf, src.ap()[base + c])
            k.op("pool", lambda e: e.dma_start(out=wb_.ap[:, 0:n], in_=src_tv.ap, max_dma_last_dim=4096),
                 reads=[src_tv], writes=[wb_], kind="d", nofence=True)

        def wload(l, name, c):
            cls, base, KT, CW = offs[l][name]
            n = KT * CW
            i = wctr[0]
            wctr[0] += 1
            if wseq is None:
                wreq.append((l, name, c))
                _wissue(i)
            else:
                assert wseq[i] == (l, name, c)
                while wiss[0] <= min(i + LA_, len(wseq) - 1):
                    _wissue(wiss[0])
                    wiss[0] += 1
            wb_ = wbf[i % NB_]
            return TV(wb_.buf, wb_.ap[:, 0:n].rearrange("p (k c) -> p k c", k=KT))

        for dt in range(8):
            for g in range(NG):
                dma(hT[dt][g], TV(io_buf, xT_d.ap()[dt, :, g * 512:(g + 1) * 512]))
        dma(gains, TV(io_buf, gains_d.ap()))
        dma(cf, TV(io_buf, cf_d.ap()))
        with contextlib.ExitStack() as ph:
            cst = sb([128, 128 * 4 + 4 * 512], F32, st=ph)
            dma(cst, TV(io_buf, cb_d.ap()))
            cp(ident, cst[:, 0:128])
            cp(ones, cst[:, 128:256])
            cp(tri, cst[:, 256:384])
            cp(negones, cst[:, 384:512])
            if n_ret > 0:
                posi = sb([128, T], I32, st=ph)
                u = sb([128, T], F32, st=ph)
                ki = sb([128, T], I32, st=ph)
                kf = sb([128, T], F32, st=ph)
                fr = sb([128, T], F32, st=ph)
                cm = sb([128, T], F32, st=ph)
                dma(posi, TV(io_buf, pos_d.ap()))
                cp(u, posi)
                ts(u, u, cf[:, 2:3], None, ALU.mult)
                ts(u, u, 1.0 / (2.0 * math.pi), None, ALU.mult)
                for which in range(2):
                    if which == 0:
                        ts(fr, u, 0.25, None, ALU.add)
                    else:
                        cp(fr, u)
                    cp(ki, fr)
                    cp(kf, ki)
                    tt(fr, fr, kf, ALU.subtract)
                    ts(cm, fr, 0.5, None, ALU.is_gt)
                    tt(fr, fr, cm, ALU.subtract)
                    ts(cm, fr, -0.5, None, ALU.is_lt)
                    tt(fr, fr, cm, ALU.add)
                    act(kf, fr, AF.Sin, scale=2.0 * math.pi)
                    dma(DT(cs_scr, cs_scr.ap()[which]), kf)
            barrier()
        ident.buf.ro = ones.buf.ro = tri.buf.ro = negones.buf.ro = True
        gains.buf.ro = cf.buf.ro = True

        negA = cf[:, 1:2]
        negB = cf[:, 3:4]

        def gcol(l, n, dt):
            c = l * 64 + n * 8 + dt
            return gains[:, c:c + 1]

        def rstd_of(srcs, ph_tiles):
            sqr, rst = ph_tiles
            ps = psum()
            for dt in range(8):
                sq = sqr[dt % len(sqr)]
                act(sq, srcs[dt], AF.Square)
                mm(ps, ones, sq, dt == 0, dt == 7)
            r = rst[0]
            rst.append(rst.pop(0))
            act(r, ps, AF.Ln, bias=RMS_EPS, scale=1.0 / D)
            act(r, r, AF.Exp, scale=-0.5)
            return r

        def prenorm(l, n, groups, xn, ph_tiles):
            for gi, g in enumerate(groups):
                r = rstd_of([hT[dt][g] for dt in range(8)], ph_tiles)
                for dt in range(8):
                    stt(xn[dt][gi], hT[dt][g], gcol(l, n, dt), r, ALU.mult, ALU.mult)

        def postnorm(l, n, g, fsb, wgt, ph_tiles, tmp):
            r = rstd_of(fsb, ph_tiles)
            for dt in range(8):
                t_ = tmp[dt % len(tmp)]
                stt(t_, fsb[dt], gcol(l, n, dt), r, ALU.mult, ALU.mult)
                stt(hT[dt][g], t_, float(wgt), hT[dt][g], ALU.mult, ALU.add)

        def norm_tiles(ph):
            return (multi([128, 512], BF16, 4, st=ph), multi([128, 512], F32, 2, st=ph))

        def ffn(l, f):
            with contextlib.ExitStack() as ph:
                nt = norm_tiles(ph)
                sqr, rst = nt
                hh = [multi([128, 512], BF16, 2, st=ph) for _ in range(NFT)]
                fsb = [multi([128, 512], F32, 2, st=ph) for _ in range(8)]
                xn = [multi([128, 512], BF16, 2, st=ph) for _ in range(8)]
                sgt = multi([128, 512], F32, 2, st=ph)
                tmp = multi([128, 512], F32, 2, st=ph)
                RING = [0, 1, 2, 3, 4]
                NBANK = [pbank[5], pbank[6]]
                n_pre, n_post = 4 * f, 4 * f + 1

                def next_r():
                    r = rst[0]
                    rst.append(rst.pop(0))
                    return r

                def pre_sq(hf):
                    for gi in range(2):
                        for dt in range(8):
                            act(xn[dt][gi], hT[dt][2 * hf + gi], AF.Square)

                def pre_mm(hf):
                    for gi in range(2):
                        for dt in range(8):
                            mm(NBANK[gi], ones, xn[dt][gi], dt == 0, dt == 7)

                def pre_fin(hf):
                    for gi in range(2):
                        g = 2 * hf + gi
                        r = next_r()
                        act(r, NBANK[gi], AF.Ln, bias=RMS_EPS, scale=1.0 / D)
                        act(r, r, AF.Exp, scale=-0.5)
                        for dt in range(8):
                            stt(xn[dt][gi], hT[dt][g], gcol(l, n_pre, dt), r, ALU.mult, ALU.mult)

                def post_sq(dt):
                    for gi in range(2):
                        act(sqr[(2 * dt + gi) % len(sqr)], fsb[dt][gi], AF.Square)

                def post_mm(dt):
                    for gi in range(2):
                        mm(NBANK[gi], ones, sqr[(2 * dt + gi) % len(sqr)], dt == 0, dt == 7)

                def post_fin(hf):
                    for gi in range(2):
                        g = 2 * hf + gi
                        r = next_r()
                        act(r, NBANK[gi], AF.Ln, bias=RMS_EPS, scale=1.0 / D)
                        act(r, r, AF.Exp, scale=-0.5)
                        for dt in range(8):
                            t_ = tmp[dt % 2]
                            stt(t_, fsb[dt][gi], gcol(l, n_post, dt), r, ALU.mult, ALU.mult)
                            stt(hT[dt][g], t_, 0.5, hT[dt][g], ALU.mult, ALU.add)

                def gateup(hf, overlap_post):
                    for c in range(11):
                        if overlap_post and c < 8:
                            post_sq(c)
                        wg = wload(l, f"g{f}", c)
                        wu = wload(l, f"u{f}", c)
                        for sub in range(2):
                            ft = 2 * c + sub
                            for gi in range(2):
                                pg = psum(RING)
                                pu = psum(RING)
                                for kt in range(8):
                                    mm(pg, wg[:, kt, sub * 128:(sub + 1) * 128], xn[kt][gi], kt == 0, kt == 7)
                                for kt in range(8):
                                    mm(pu, wu[:, kt, sub * 128:(sub + 1) * 128], xn[kt][gi], kt == 0, kt == 7)
                                s_ = sgt[(ft * 2 + gi) % 2]
                                act(s_, pg, AF.Silu)
                                tt(hh[ft][gi], s_, pu, ALU.mult)
                        if overlap_post and c < 8:
                            post_mm(c)
                        if overlap_post and c == 8:
                            post_fin(hf - 1)

                def down(hf, overlap_pre):
                    for mt in range(8):
                        if overlap_pre and mt == 0:
                            pre_sq(hf + 1)
                        wd = wload(l, f"d{f}", mt)
                        for gi in range(2):
                            pf = psum(RING)
                            for ft in range(NFT):
                                mm(pf, wd[:, ft, :], hh[ft][gi], ft == 0, ft == NFT - 1)
                            act(fsb[mt][gi], pf, AF.Copy)
                        if overlap_pre and mt == 1:
                            pre_mm(hf + 1)
                        if overlap_pre and mt == 2:
                            pre_fin(hf + 1)

                pre_sq(0)
                pre_mm(0)
                pre_fin(0)
                gateup(0, False)
                down(0, True)
                gateup(1, True)
                down(1, False)
                for dt in range(8):
                    post_sq(dt)
                    post_mm(dt)
                post_fin(1)
                barrier()

        def ple(l):
            with contextlib.ExitStack() as ph:
                nt = norm_tiles(ph)
                xn = [multi([128, 512], BF16, 2, st=ph) for _ in range(8)]
                fsb = [multi([128, 512], F32, 2, st=ph) for _ in range(8)]
                sgt = multi([128, 512], F32, 2, st=ph)
                tmp = multi([128, 512], F32, 2, st=ph)
                pf32 = multi([128, 1024], F32, 2, st=ph)
                pb = multi([128, 1024], BF16, 2, st=ph)
                for hf in range(2):
                    groups = [2 * hf, 2 * hf + 1]
                    for kt in range(2):
                        dma(pf32[kt], TV(io_buf, pT_d.ap()[l, kt, :, hf * 1024:(hf + 1) * 1024]))
                        cp(pb[kt], pf32[kt], eng="pool")
                    prenorm(l, 6, groups, xn, nt)
                    for c in range(4):
                        wp = wload(l, "pp", 0)
                        wg = wload(l, "pg", c)
                        for sub in range(2):
                            mt = 2 * c + sub
                            for gi in range(2):
                                pg = psum()
                                pe_ = psum()
                                for kt in range(8):
                                    mm(pg, wg[:, kt, sub * 128:(sub + 1) * 128], xn[kt][gi], kt == 0, kt == 7)
                                for kt in range(2):
                                    mm(pe_, wp[:, kt, mt * 128:(mt + 1) * 128], pb[kt][:, gi * 512:(gi + 1) * 512],
                                       kt == 0, kt == 1)
                                s_ = sgt[(mt * 2 + gi) % 2]
                                act(s_, pg, AF.Sigmoid)
                                tt(fsb[mt][gi], s_, pe_, ALU.mult)
                    for gi in range(2):
                        postnorm(l, 7, groups[gi], [fsb[mt][gi] for mt in range(8)], 1.0, nt, tmp)
                barrier()

        def sb_mixer(l):
            with contextlib.ExitStack() as ph:
                nt = norm_tiles(ph)
                xn = [multi([128, 512], BF16, 2, st=ph) for _ in range(8)]
                stq = multi([128, 1024], BF16, 4, st=ph)
                vst = multi([128, 8, 128], BF16, 4, st=ph)
                sc = 0
                for hf in range(2):
                    groups = [2 * hf, 2 * hf + 1]
                    prenorm(l, 2, groups, xn, nt)
                    for qk in range(2):
                        for c in range(4):
                            w = wload(l, "min", qk * 4 + c)
                            for sub in range(2):
                                h = 2 * c + sub
                                st_ = stq[sc % 4]
                                sc += 1
                                for gi in range(2):
                                    ps = psum()
                                    for kt in range(8):
                                        mm(ps, w[:, kt, sub * 128:(sub + 1) * 128], xn[kt][gi], kt == 0, kt == 7)
                                    act(st_[:, gi * 512:(gi + 1) * 512], ps, AF.Copy,
                                        scale=(SDH ** -0.5 if qk == 0 else 1.0))
                                if qk == 0:
                                    dma(DT(qscr[h], qscr[h].ap()[:, hf * 1024:(hf + 1) * 1024]), st_)
                                else:
                                    dma(DT(xs[h], xs[h].ap()[0:128, hf * 1024:(hf + 1) * 1024]), st_)
                    for c in range(4):
                        w = wload(l, "min", 8 + c)
                        va = vst[(2 * c) % 4]
                        vb = vst[(2 * c + 1) % 4]
                        for b in range(8):
                            gi, bb = b // 4, b % 4
                            ps = psum()
                            for kt in range(8):
                                mm(ps[:, 0:256], xn[kt][gi][:, bb * 128:(bb + 1) * 128], w[:, kt, :], kt == 0, kt == 7)
                            act(va[:, b, :], ps[:, 0:128], AF.Copy)
                            act(vb[:, b, :], ps[:, 128:256], AF.Copy)
                        for sub, vv in ((0, va), (1, vb)):
                            h = 2 * c + sub
                            dma(DT(xs[h], xs[h].ap()[128:256, hf * 1024:(hf + 1) * 1024].rearrange(
                                "p (b d) -> p b d", b=8)), vv)
                barrier()
            for h in range(SH):
                s_, d_ = DT(xs[h]), DT(xd[h])
                k.op("pool", lambda e, s_=s_, d_=d_: e.collective_compute(
                    "AllGather", ALU.bypass, replica_groups=[[0, 1], [2, 3], [4, 5], [6, 7]],
                    ins=[s_.ap.opt()], outs=[d_.ap.opt()]), reads=[s_], writes=[d_], kind="cc")
            with contextlib.ExitStack() as ph:
                nt = norm_tiles(ph)
                oT = [multi([128, 512], BF16, NG, st=ph) for _ in range(SH)]
                qt = multi([128, T], BF16, 1, st=ph)
                kall = multi([128, 3 * T], BF16, 1, st=ph)
                vall = multi([128, 48, 128], BF16, 1, st=ph)
                et = multi([128, 512], F32, 2, st=ph)
                spt = multi([128, 512], BF16, 3, st=ph)
                at = multi([128, 512], BF16, 2, st=ph)
                lsum = multi([128, 512], BF16, 3, st=ph)
                fsb = multi([128, 512], F32, 8, st=ph)
                tmp = et
                maskb = sb([128, 4, 512], BF16, st=ph)
                for kl in range(4):
                    dma(et[kl % 2], TV(io_buf, cb_d.ap()[:, 512 + kl * 512:512 + (kl + 1) * 512]))
                    cp(maskb[:, kl, :], et[kl % 2])
                tc_ = 0
                for h in range(SH):
                    q_ = qt[0]
                    K_ = kall[0]
                    V_ = vall[0]
                    dma(q_, DT(qscr[h]))
                    dma(K_[:, 0:T], DT(xd[h], xd[h].ap()[0:128, :]))
                    dma(K_[:, T:2 * T], DT(xd[h], xd[h].ap()[256:384, :]))
                    dma(K_[:, 2 * T:3 * T], DT(xs[h], xs[h].ap()[0:128, :]))
                    dma(V_[:, 0:16, :], DT(xd[h], xd[h].ap()[128:256, :].rearrange("p (b d) -> p b d", b=16)))
                    dma(V_[:, 16:32, :], DT(xd[h], xd[h].ap()[384:512, :].rearrange("p (b d) -> p b d", b=16)))
                    dma(V_[:, 32:48, :], DT(xs[h], xs[h].ap()[128:256, :].rearrange("p (b d) -> p b d", b=16)))
                    flat = []
                    for qg in range(NG):
                        tiles = [(32 + 4 * qg + kl, kl, None) for kl in range(3, -1, -1)]
                        for rk, lg, msk in SB_PRIORS[qg]:
                            tiles += [(16 * rk + 4 * lg + bb, None, msk) for bb in range(3, -1, -1)]
                        for ti, (kb, kl, prev) in enumerate(tiles):
                            prev = {None: None, 'A': negA, 'B': negB}[prev]
                            flat.append(dict(qg=qg, kb=kb, kl=kl, prev=prev, first=ti == 0,
                                             last=ti == len(tiles) - 1, po=pbank[(h * NG + qg) % 2],
                                             L=lsum[tc_ % 3], Ln=lsum[(tc_ + 1) % 3], e=et[tc_ % 2],
                                             s=spt[tc_ % 3], a=at[tc_ % 2], pz=pbank[2 + tc_ % 4]))
                            tc_ += 1

                    def stageA(t):
                        qv = q_[:, t["qg"] * 512:(t["qg"] + 1) * 512]
                        kv = K_[:, t["kb"] * 128:(t["kb"] + 1) * 128]
                        pz = t["pz"]
                        mm(pz, kv, qv, True, t["kl"] is None)
                        if t["kl"] is not None:
                            mm(pz, ident, maskb[:, t["kl"], :], False, True)
                        act(t["e"], pz, AF.Exp, bias=t["prev"])
                        act(t["s"], t["e"], AF.Ln, bias=1.0)
                        if not t["last"]:
                            if t["first"]:
                                cp(t["Ln"], t["s"])
                            else:
                                tt(t["Ln"], t["L"], t["s"], ALU.add)

                    def stageB1(t):
                        pz = t["pz"]
                        mmx(pz, tri, t["s"], False, t["first"])
                        if not t["first"]:
                            mmx(pz, negones, t["L"], False, True)
                        act(t["a"], pz, AF.Exp, bias=t["prev"])

                    def stageB2(t):
                        mm(t["po"], V_[:, t["kb"], :], t["a"], t["first"], t["last"])
                        if t["last"]:
                            cp(oT[h][t["qg"]], t["po"])

                    n_ = len(flat)
                    for i in range(-2, n_):
                        if 0 <= i + 2 < n_:
                            stageA(flat[i + 2])
                        if 0 <= i + 1 < n_:
                            stageB1(flat[i + 1])
                        if i >= 0:
                            stageB2(flat[i])
                for g in range(NG):
                    for c in range(4):
                        w = wload(l, "mout", c)
                        for sub in range(2):
                            mt = 2 * c + sub
                            ps = psum([2, 3, 4, 5, 6])
                            for hh_ in range(SH):
                                mm(ps, w[:, hh_, sub * 128:(sub + 1) * 128], oT[hh_][g], hh_ == 0, hh_ == SH - 1)
                            act(fsb[mt], ps, AF.Copy)
                    postnorm(l, 3, g, fsb, 1.0, nt, tmp)
                barrier()

        def ret_mixer(l, j):
            rcb = [None]
            mask_of = lambda h: rcb[0][:, h * 128:(h + 1) * 128]
            xi_of = lambda h: rcb[0][:, 512 + h:513 + h]
            zeta_of = lambda h: rcb[0][:, 516 + h:517 + h]
            zeta2_of = lambda tot, h, c: cf[:, 32 + tot * 64 + h * 16 + c:33 + tot * 64 + h * 16 + c]
            ccol = lambda g, h: cf[:, 8 + g * 4 + h:9 + g * 4 + h]
            dcol = lambda g: cf[:, 24 + g:25 + g]

            def load_rconst(ph):
                rcb[0] = sb([128, 4 * 128 + 4 + 4 + 64], F32, st=ph)
                dma(rcb[0], TV(io_buf, rc_d.ap()))

            def rope(dst, p1, p2, cos_, sin_, tm):
                tt(tm[0], p1, cos_, ALU.mult)
                tt(tm[1], p2, sin_, ALU.mult)
                tt(dst[0], tm[0], tm[1], ALU.subtract)
                tt(tm[2], p1, sin_, ALU.mult)
                tt(tm[3], p2, cos_, ALU.mult)
                tt(dst[1], tm[2], tm[3], ALU.add)

            with contextlib.ExitStack() as ph:
                nt = norm_tiles(ph)
                load_rconst(ph)
                xn = [multi([128, 512], BF16, NG, st=ph) for _ in range(8)]
                cs = multi([128, 512], F32, 4, st=ph)
                krT = multi([128, T], BF16, 2, st=ph)
                vh = sb([128, 16, 512], BF16, st=ph)
                tm = multi([128, 512], F32, 4, st=ph)
                kz = multi([128, 256], BF16, 4, st=ph)
                sev = multi([128, 512], F32, 2, st=ph)
                prenorm(l, 2, [0, 1, 2, 3], xn, nt)
                for h in range(RH):
                    w = wload(l, "min", 4 + h)
                    for g in range(NG):
                        p1, p2 = psum([4, 5, 6]), psum([4, 5, 6])
                        for kt in range(8):
                            mm(p1, w[:, kt, 0:128], xn[kt][g], kt == 0, kt == 7)
                        for kt in range(8):
                            mm(p2, w[:, kt, 128:256], xn[kt][g], kt == 0, kt == 7)
                        sl = slice(g * 512, (g + 1) * 512)
                        c0, c1 = cs[(g % 2) * 2], cs[(g % 2) * 2 + 1]
                        dma(c0, DT(cs_scr, cs_scr.ap()[0, :, sl]))
                        dma(c1, DT(cs_scr, cs_scr.ap()[1, :, sl]))
                        rope([krT[0][:, sl], krT[1][:, sl]], p1, p2, c0, c1, tm)
                    for cc_ in range(2):
                        w = wload(l, "min", 8 + 2 * h + cc_)
                        for b in range(16):
                            g, bb = b // 4, b % 4
                            ps = psum([4, 5, 6])
                            for kt in range(8):
                                mm(ps[:, 0:256], xn[kt][g][:, bb * 128:(bb + 1) * 128], w[:, kt, :], kt == 0, kt == 7)
                            act(vh[:, b, cc_ * 256:(cc_ + 1) * 256], ps[:, 0:256], AF.Copy)
                    for b in range(16):
                        pt = ptrans()
                        for a in range(2):
                            transpose(pt[:, a * 128:(a + 1) * 128], krT[a][:, b * 128:(b + 1) * 128])
                        for tot in range(2):
                            kz_ = kz[(2 * b + tot) % 4]
                            act(kz_, pt, AF.Copy, scale=zeta2_of(tot, h, b))
                            for a in range(2):
                                mm(pbank[2 * tot + a], kz_[:, a * 128:(a + 1) * 128], vh[:, b, :], b == 0, b == 15)
                    for tot in range(2):
                        for a in range(2):
                            i = tot * 8 + h * 2 + a
                            sv = sev[(2 * tot + a) % 2]
                            cp(sv, pbank[2 * tot + a])
                            dma(DT(sxs[i // 4], sxs[i // 4].ap()[(i % 4) * 128:(i % 4 + 1) * 128, :]), sv)
                barrier()
            for i in range(4):
                s_, d_ = DT(sxs[i]), DT(sxd[i])
                k.op("pool", lambda e, s_=s_, d_=d_: e.collective_compute(
                    "AllGather", ALU.bypass, replica_groups=[[0, 1], [2, 3], [4, 5], [6, 7]],
                    ins=[s_.ap.opt()], outs=[d_.ap.opt()]), reads=[s_], writes=[d_], kind="cc")
            with contextlib.ExitStack() as ph:
                nt = norm_tiles(ph)
                load_rconst(ph)
                ogT = multi([128, 512], BF16, 16, st=ph)
                xn = [[ogT[8 + dt]] for dt in range(8)]
                cs = multi([128, 512], F32, 2, st=ph)
                LN = 2
                qrT = [multi([128, 512], BF16, 2, st=ph) for _ in range(LN)]
                krT = [multi([128, 512], BF16, 2, st=ph) for _ in range(LN)]
                vh = [sb([128, 4, 512], BF16, st=ph) for _ in range(LN)]
                sgh = [sb([128, 4, 512], BF16, st=ph) for _ in range(LN)]
                gnt = multi([128, 512], F32, 2, st=ph)
                S = [multi([128, 512], F32, 2, st=ph) for _ in range(RH)]
                Sbf = [multi([128, 512], BF16, 2, st=ph) for _ in range(LN)]
                big = multi([128, 512], F32, 10, st=ph)
                tm = big[0:4]
                osb = [big[4:7], big[7:10]]
                scm = [multi([128, 128], BF16, 2, st=ph) for _ in range(LN)]
                kz = [multi([128, 256], BF16, 2, st=ph) for _ in range(LN)]
                og = [multi([128, 512], BF16, 2, st=ph) for _ in range(LN)]
                st6 = [multi([128, 6], F32, 2, st=ph) for _ in range(LN)]
                mv = [multi([128, 2], F32, 2, st=ph) for _ in range(LN)]
                rs = [multi([128, 1], F32, 2, st=ph) for _ in range(LN)]
                fsb = big[0:8]
                tmp = gnt
                ring = [2, 3, 4, 5, 6]

                def project(g, h, ln):
                    for qk, dst in ((0, qrT[ln]), (1, krT[ln])):
                        w = wload(l, "min", qk * 4 + h)
                        p1, p2 = psum(ring), psum(ring)
                        for kt in range(8):
                            mm(p1, w[:, kt, 0:128], xn[kt][0], kt == 0, kt == 7)
                        for kt in range(8):
                            mm(p2, w[:, kt, 128:256], xn[kt][0], kt == 0, kt == 7)
                        rope(dst, p1, p2, cs[0], cs[1], tm)
                    gn_ = gnt[ln]
                    dma(gn_, TV(io_buf, gn_d.ap()[j * RH + h]))
                    for vg, dstt in ((0, vh[ln]), (1, sgh[ln])):
                        for cc_ in range(2):
                            w = wload(l, "min", 8 + vg * 8 + 2 * h + cc_)
                            for b in range(4):
                                ps = psum(ring)
                                for kt in range(8):
                                    mm(ps[:, 0:256], xn[kt][0][:, b * 128:(b + 1) * 128], w[:, kt, :],
                                       kt == 0, kt == 7)
                                if vg == 0:
                                    act(dstt[:, b, cc_ * 256:(cc_ + 1) * 256], ps[:, 0:256], AF.Copy)
                                else:
                                    t_ = tm[(b + cc_) % 4]
                                    act(t_[:, 0:256], ps[:, 0:256], AF.Silu)
                                    tt(dstt[:, b, cc_ * 256:(cc_ + 1) * 256], t_[:, 0:256],
                                       gn_[:, cc_ * 256:(cc_ + 1) * 256], ALU.mult)
                    for a in range(2):
                        i = (g // 2) * 8 + h * 2 + a
                        r0 = (g % 2) * 512 + (i % 4) * 128
                        dma(tm[a], DT(sxd[i // 4], sxd[i // 4].ap()[r0:r0 + 128, :]))
                        if g == 0:
                            ts(S[h][a], tm[a], dcol(g), None, ALU.mult)
                        else:
                            ts(tm[a], tm[a], dcol(g), None, ALU.mult)
                            stt(S[h][a], S[h][a], ccol(g, h), tm[a], ALU.mult, ALU.add)
                    for a in range(2):
                        act(Sbf[ln][a], S[h][a], AF.Copy)

                def lane_stages(h, ln):
                    po = pbank[ln]

                    def m1(c):
                        bs = slice(c * 128, (c + 1) * 128)
                        psc = psum(ring)
                        for a in range(2):
                            mm(psc[:, 0:128], krT[ln][a][:, bs], qrT[ln][a][:, bs], a == 0, a == 1)
                        tt(scm[ln][c % 2], psc[:, 0:128], mask_of(h), ALU.mult)

                    def m3(c):
                        bs = slice(c * 128, (c + 1) * 128)
                        mm(po, scm[ln][c % 2], vh[ln][:, c, :], True, False)
                        for a in range(2):
                            mm(po, qrT[ln][a][:, bs], Sbf[ln][a], False, a == 1)
                        pt = ptrans()
                        for a in range(2):
                            transpose(pt[:, a * 128:(a + 1) * 128], krT[ln][a][:, bs])
                        act(kz[ln][c % 2], pt, AF.Copy, scale=zeta_of(h))
                        act(osb[ln][c % 3], po, AF.Copy, scale=xi_of(h))

                    def m5(c):
                        kz_ = kz[ln][c % 2]
                        pS = [psum(ring), psum(ring)]
                        for a in range(2):
                            mm(pS[a], kz_[:, a * 128:(a + 1) * 128], vh[ln][:, c, :], True, True)
                        for a in range(2):
                            stt(S[h][a], S[h][a], float(GAMMA[h] ** 128), pS[a], ALU.mult, ALU.add)

                    def m7(c):
                        for a in range(2):
                            act(Sbf[ln][a], S[h][a], AF.Copy)

                    def n1(c):
                        o_, s6, mv_ = osb[ln][c % 3], st6[ln][c % 2], mv[ln][c % 2]
                        k.op("dve", lambda e: e.bn_stats(out=s6.ap, in_=o_.ap), reads=[o_], writes=[s6])
                        k.op("dve", lambda e: e.bn_aggr(out=mv_.ap, in_=s6.ap), reads=[s6], writes=[mv_])

                    def n2(c):
                        mv_, rs_ = mv[ln][c % 2], rs[ln][c % 2]
                        act(rs_, mv_[:, 1:2], AF.Ln, bias=GN_EPS)
                        act(rs_, rs_, AF.Exp, scale=-0.5)

                    def n3(c):
                        o_, mv_, rs_, og_ = osb[ln][c % 3], mv[ln][c % 2], rs[ln][c % 2], og[ln][c % 2]
                        ts(o_, o_, mv_[:, 0:1], rs_, ALU.subtract, ALU.mult)
                        tt(og_, o_, sgh[ln][:, c, :], ALU.mult)

                    def n4(c):
                        bs = slice(c * 128, (c + 1) * 128)
                        og_ = og[ln][c % 2]
                        for half in range(2):
                            pt2 = ptrans()
                            for q2 in range(2):
                                q4 = half * 2 + q2
                                transpose(pt2[:, q2 * 128:(q2 + 1) * 128], og_[:, q4 * 128:(q4 + 1) * 128])
                            for q2 in range(2):
                                q4 = half * 2 + q2
                                cp(ogT[h * 4 + q4][:, bs], pt2[:, q2 * 128:(q2 + 1) * 128])

                    st = []

                    def add(fn, c):
                        if 0 <= c < 4:
                            st.append(lambda fn=fn, c=c: fn(c))

                    for c in range(6):
                        add(m1, c)
                        add(m3, c)
                        add(n1, c - 1)
                        add(m5, c)
                        add(n2, c - 1)
                        add(n4, c - 2)
                        add(m7, c)
                        add(n3, c - 1)
                    return st

                for g in range(NG):
                    sl = slice(g * 512, (g + 1) * 512)
                    dma(cs[0], DT(cs_scr, cs_scr.ap()[0, :, sl]))
                    dma(cs[1], DT(cs_scr, cs_scr.ap()[1, :, sl]))
                    prenorm(l, 2, [g], xn, nt)
                    for pair in range(2):
                        hs = (2 * pair, 2 * pair + 1)
                        for ln, h in enumerate(hs):
                            project(g, h, ln)
                        lists = [lane_stages(h, ln) for ln, h in enumerate(hs)]
                        if ZIP_LANES:
                            for i in range(max(len(x) for x in lists)):
                                for lst in lists:
                                    if i < len(lst):
                                        lst[i]()
                        else:
                            for lst in lists:
                                for fn_ in lst:
                                    fn_()
                    for mt in range(8):
                        w = wload(l, "mout", mt)
                        ps = psum(ring)
                        for kt in range(16):
                            mm(ps, w[:, kt, :], ogT[kt], kt == 0, kt == 15)
                        act(fsb[mt], ps, AF.Copy)
                    postnorm(l, 3, g, fsb, 1.0, nt, tmp)
                barrier()

        jr = 0
        for l, kind in enumerate(kinds):
            ffn(l, 0)
            if kind == 'ret':
                ret_mixer(l, jr)
                jr += 1
            else:
                sb_mixer(l)
            ffn(l, 1)
            ple(l)
        for dt in range(8):
            for g in range(NG):
                o = dma(TV(Buf(), yT_d.ap()[dt, :, g * 512:(g + 1) * 512]), hT[dt][g])
                k.out_dmas.append(o)
        k.emit(stack)
    return nc, wreq


_NC_CACHE = {}


def _get_nc(kinds):
    key = tuple(kinds)
    if key not in _NC_CACHE:
        _, seq = build(list(kinds))
        _NC_CACHE[key] = build(list(kinds), wseq=seq)[0]
    return _NC_CACHE[key]


def _const_inputs():
    f32 = np.float32
    rc = np.zeros((128, 4 * 128 + 4 + 4 + 64), f32)
    jj = np.arange(128, dtype=np.float64)
    for h in range(RH):
        lg = math.log1p(-2.0 ** (-5.0 - h))
        m = np.zeros((128, 128), np.float64)
        for j in range(128):
            m[j, j:] = (RDK ** -0.5) * math.exp(-(j + 1) * lg)
        rc[:, h * 128:(h + 1) * 128] = m
        rc[:, 512 + h] = np.exp((jj + 1.0) * lg)
        rc[:, 516 + h] = (RDK ** -0.5) * np.exp((127.0 - jj) * lg)
        for c in range(16):
            rc[:, 520 + h * 16 + c] = (RDK ** -0.5) * np.exp((2047.0 - (c * 128 + jj)) * lg)
    cb = np.zeros((128, 128 * 4 + 4 * 512), f32)
    cb[:, 0:128] = np.eye(128)
    cb[:, 128:256] = 1.0
    s = np.arange(128)
    cb[:, 256:384] = -(s[:, None] >= s[None, :]).astype(f32)
    cb[:, 384:512] = -1.0
    t = np.arange(512)
    for kl in range(4):
        ok = (kl * 128 + s[:, None]) < t[None, :]
        cb[:, 512 + kl * 512:512 + (kl + 1) * 512] = np.where(ok, 0.0, NEG)
    return rc, cb


def _prep_shared(kinds, lids, inp):
    offs, NA, NBk = layer_offsets(kinds)
    wA = np.zeros((NA, 128, 2048), np.float32)
    wB = np.zeros((max(NBk, 1), 128, 2816), np.float32)
    gains = np.zeros((128, len(kinds) * 64), np.float32)
    n_ret = sum(1 for x in kinds if x == 'ret')
    gnb = np.zeros((max(n_ret, 1) * RH, 128, 512), np.float32)
    jr = 0
    for li, (kind, L) in enumerate(zip(kinds, lids)):
        o = offs[li]

        def put(name, W):
            cls, base, KT, CW = o[name]
            pk = pack_w(np.asarray(W, np.float32), KT, CW)
            (wA if cls == 'A' else wB)[base:base + pk.shape[0]] = pk
        for f in range(2):
            put(f"g{f}", inp["ffn_w_gate"][L, f])
            put(f"u{f}", inp["ffn_w_up"][L, f])
            put(f"d{f}", inp["ffn_w_down"][L, f])
        j = L // 2
        if kind == 'ret':
            put("min", inp["ret_w_in"][j])
            put("mout", inp["ret_w_out"][j])
            gg = np.asarray(inp["ret_gn_gain"][j], np.float32).reshape(RH, 1, 512)
            gnb[jr * RH:(jr + 1) * RH] = np.broadcast_to(gg, (RH, 128, 512))
            jr += 1
        else:
            put("min", inp["sb_w_in"][j])
            put("mout", inp["sb_w_out"][j])
        put("pg", inp["ple_w_gate"][L])
        put("pp", inp["ple_w_proj"][L])
        g = np.asarray(inp["norm_gains"][L], np.float32)
        gains[:, li * 64:(li + 1) * 64] = g.reshape(8, 8, 128).transpose(2, 0, 1).reshape(128, 64)
    return wA, wB, gains, gnb


def _run(kinds, lids, inp, hT_cores):
    nc = _get_nc(kinds)
    wA, wB, gains, gnb = _prep_shared(kinds, lids, inp)
    rc, cb = _const_inputs()
    p = np.asarray(inp["p"], np.float32)
    pos = np.asarray(inp["positions"]).astype(np.int32)
    inv = (10000.0 ** (-np.arange(128, dtype=np.float32) / np.float32(128))).astype(np.float32)
    in_maps = []
    jj = np.arange(128, dtype=np.float64)
    for c in range(8):
        b, hf = c // 2, c % 2
        tok = tok_idx(hf)
        pT = np.stack([np.ascontiguousarray(p[L, b, tok].T).reshape(2, 128, T) for L in lids])
        cf = np.zeros((128, 160), np.float32)
        cf[:, 0] = float(hf)
        cf[:, 1] = NEG if hf == 0 else 0.0
        cf[:, 2] = inv
        cf[:, 3] = NEG if hf == 1 else 0.0
        for h in range(RH):
            lg = math.log1p(-2.0 ** (-5.0 - h))
            g1024 = math.exp(1024.0 * lg)
            cvals = (0.0, g1024, 1.0, g1024) if hf == 0 else (0.0, 1.0, g1024, 1.0)
            for g in range(4):
                cf[:, 8 + g * 4 + h] = cvals[g]
            rng = ((0, 3, 511.0), (4, 11, 1535.0)) if hf == 0 else ((0, 7, 1023.0), (8, 15, 2047.0))
            for tot in range(2):
                b0, b1, tend = rng[tot]
                for blk in range(b0, b1 + 1):
                    cf[:, 32 + tot * 64 + h * 16 + blk] = (RDK ** -0.5) * np.exp((tend - (blk * 128 + jj)) * lg)
        dvals = (0.0, 1.0, 0.0, 1.0) if hf == 0 else (1.0, 0.0, 1.0, 0.0)
        for g in range(4):
            cf[:, 24 + g] = dvals[g]
        in_maps.append({
            "xT": hT_cores[c], "pT": pT,
            "pos": np.ascontiguousarray(np.broadcast_to(pos[b][tok][None, :], (128, T))),
            "wA": wA, "wB": wB, "gains": gains, "gnb": gnb, "cf": cf, "rconst": rc, "cbf": cb,
        })
    res = run_bass_kernel_spmd(nc, in_maps, core_ids=list(range(8)), **RUN_KW)
    return [np.asarray(res.results[c]["yT"]) for c in range(8)]


FUSED = True
RUN_KW = {}
ZIP_LANES = False


def tok_idx(rank):
    return np.concatenate([np.arange(g * 512, (g + 1) * 512) for g in OWN[rank]])


def shard_x(x):
    return [np.ascontiguousarray(x[c // 2, tok_idx(c % 2)].T).reshape(8, 128, T) for c in range(8)]


def unshard(hT):
    out = np.zeros((BATCH, SEQ, D), np.float32)
    for c in range(8):
        out[c // 2, tok_idx(c % 2)] = hT[c].reshape(D, T).T
    return out


def kernel(**inp):
    x = np.asarray(inp["x"], np.float32)
    hT = shard_x(x)
    all_kinds = ['ret' if i % 2 == 0 else 'sb' for i in range(DEPTH)]
    if FUSED:
        hT = _run(all_kinds, list(range(DEPTH)), inp, hT)
    else:
        for L in range(DEPTH):
            hT = _run([all_kinds[L]], [L], inp, hT)
    return unshard(hT)
```
